# Optimizing a Trainium2 kernel written in Bass

```python
import math
import jax, jax.numpy as jnp
from jax import lax
import numpy as np

D_MODEL = 1024
BATCH = 8
SEQ = 8192
DEPTH = 2

RET_HEADS = 4
RET_QK_DIM = D_MODEL // RET_HEADS
RET_V_DIM = 2 * RET_QK_DIM
RET_QK_TOTAL = RET_HEADS * RET_QK_DIM
RET_V_TOTAL = RET_HEADS * RET_V_DIM
RET_IN_COLS = 2 * RET_QK_TOTAL + 2 * RET_V_TOTAL
RET_CHUNK = 128
ROPE_BASE = 10000.0
CONV_WIDTH = 3
D_FF = 7 * D_MODEL // 2
N_EXPERTS = 8
TOP_K = 2
NORM_EPS = 1e-6

kernel_name = "hybrid_retention_shortconv_moe"


def _rmsnorm(x, g):
    xf = x.astype(jnp.float32)
    y = xf * lax.rsqrt(jnp.mean(xf * xf, axis=-1, keepdims=True) + NORM_EPS)
    return (y * g.astype(jnp.float32)).astype(x.dtype)


def _rotary(t, pos):
    d = t.shape[-1]
    inv_freq = ROPE_BASE ** (-jnp.arange(0, d, 2, dtype=jnp.float32) / d)
    ang = pos[:, None] * inv_freq[None, :]
    cos = jnp.cos(ang)[None, :, None, :]
    sin = jnp.sin(ang)[None, :, None, :]
    t1, t2 = t[..., : d // 2], t[..., d // 2:]
    return jnp.concatenate([t1 * cos - t2 * sin, t1 * sin + t2 * cos], axis=-1)


def _retention(xn, w_in, w_out):
    b, s, _ = xn.shape
    proj = (xn @ w_in).astype(jnp.float32)
    q = proj[..., :RET_QK_TOTAL].reshape(b, s, RET_HEADS, RET_QK_DIM)
    k = proj[..., RET_QK_TOTAL:2 * RET_QK_TOTAL].reshape(b, s, RET_HEADS, RET_QK_DIM)
    v = proj[..., 2 * RET_QK_TOTAL:2 * RET_QK_TOTAL + RET_V_TOTAL].reshape(b, s, RET_HEADS, RET_V_DIM)
    g = proj[..., 2 * RET_QK_TOTAL + RET_V_TOTAL:]
    pos = jnp.arange(s, dtype=jnp.float32)
    q = _rotary(q, pos)
    k = _rotary(k, pos) * (RET_QK_DIM ** -0.5)

    nc = s // RET_CHUNK

    def to_chunks(t):
        return t.reshape(b, nc, RET_CHUNK, RET_HEADS, t.shape[-1]).transpose(1, 0, 3, 2, 4)

    qc, kc, vc = to_chunks(q), to_chunks(k), to_chunks(v)

    log_gamma = jnp.log(1.0 - 2.0 ** (-5.0 - jnp.arange(RET_HEADS, dtype=jnp.float32)))
    idx = jnp.arange(RET_CHUNK, dtype=jnp.float32)
    diff = idx[:, None] - idx[None, :]
    causal = diff >= 0
    intra_decay = jnp.where(causal[None], jnp.exp(jnp.where(causal, diff, 0.0)[None] * log_gamma[:, None, None]), 0.0)
    q_decay = jnp.exp((idx[None, :] + 1.0) * log_gamma[:, None])
    k_decay = jnp.exp((RET_CHUNK - 1.0 - idx[None, :]) * log_gamma[:, None])
    chunk_decay = jnp.exp(RET_CHUNK * log_gamma)

    def step(state, inp):
        q_i, k_i, v_i = inp
        scores = jnp.einsum('bhid,bhjd->bhij', q_i, k_i) * intra_decay[None]
        inner = jnp.einsum('bhij,bhje->bhie', scores, v_i)
        cross = jnp.einsum('bhid,bhde->bhie', q_i * q_decay[None, :, :, None], state)
        new_state = state * chunk_decay[None, :, None, None] + jnp.einsum(
            'bhjd,bhje->bhde', k_i * k_decay[None, :, :, None], v_i)
        return new_state, inner + cross

    state0 = jnp.zeros((b, RET_HEADS, RET_QK_DIM, RET_V_DIM), jnp.float32)
    _, o = lax.scan(step, state0, (qc, kc, vc))
    o = o.transpose(1, 0, 3, 2, 4).reshape(b, s, RET_HEADS, RET_V_DIM)
    o = o * lax.rsqrt(jnp.mean(o * o, axis=-1, keepdims=True) + NORM_EPS)
    o = jax.nn.silu(g) * o.reshape(b, s, RET_V_TOTAL)
    return o.astype(xn.dtype) @ w_out


def _short_conv(xn, w_in, w_conv, w_out):
    s = xn.shape[1]
    proj = xn @ w_in
    gate_b = proj[..., :D_MODEL]
    gate_c = proj[..., D_MODEL:2 * D_MODEL]
    h = proj[..., 2 * D_MODEL:]
    u = (gate_c * h).astype(jnp.float32)
    u_pad = jnp.pad(u, ((0, 0), (CONV_WIDTH - 1, 0), (0, 0)))
    wc = w_conv.astype(jnp.float32)
    y = sum(wc[j][None, None, :] * u_pad[:, j:j + s, :] for j in range(CONV_WIDTH))
    y = gate_b.astype(jnp.float32) * y
    return y.astype(xn.dtype) @ w_out


def _swiglu(xn, w_gate, w_up, w_down):
    return (jax.nn.silu(xn @ w_gate) * (xn @ w_up)) @ w_down


def _moe_swiglu(xn, w_router, w_gate_e, w_up_e, w_down_e):
    b, s, d = xn.shape
    t = xn.reshape(b * s, d)
    logits = (t @ w_router).astype(jnp.float32)
    top_vals, top_idx = lax.top_k(logits, TOP_K)
    top_w = jax.nn.softmax(top_vals, axis=-1)
    gates = jnp.sum(jax.nn.one_hot(top_idx, N_EXPERTS, dtype=jnp.float32) * top_w[..., None], axis=1)
    out = jnp.zeros((b * s, d), jnp.float32)
    for e in range(N_EXPERTS):
        h = jax.nn.silu(t @ w_gate_e[e]) * (t @ w_up_e[e])
        out = out + gates[:, e:e + 1] * (h @ w_down_e[e]).astype(jnp.float32)
    return out.astype(xn.dtype).reshape(b, s, d)


def setup_inputs(seed: int = 0) -> dict:
    key = jax.random.key(seed)
    ks = jax.random.split(key, 24)

    def w(k, shape, fan_in):
        return jax.random.normal(k, shape, jnp.float32) * (fan_in ** -0.5)

    def gain(k):
        return 1.0 + 0.02 * jax.random.normal(k, (D_MODEL,), jnp.float32)

    return {
        "x": jax.random.normal(ks[0], (BATCH, SEQ, D_MODEL), jnp.float32),
        "norm_mix0": gain(ks[1]),
        "ret_w_in": w(ks[2], (D_MODEL, RET_IN_COLS), D_MODEL),
        "ret_w_out": w(ks[3], (RET_V_TOTAL, D_MODEL), RET_V_TOTAL),
        "norm_ffn0": gain(ks[4]),
        "ffn_w_gate": w(ks[5], (D_MODEL, D_FF), D_MODEL),
        "ffn_w_up": w(ks[6], (D_MODEL, D_FF), D_MODEL),
        "ffn_w_down": w(ks[7], (D_FF, D_MODEL), D_FF),
        "norm_mix1": gain(ks[8]),
        "conv_w_in": w(ks[9], (D_MODEL, 3 * D_MODEL), D_MODEL),
        "conv_w": w(ks[10], (CONV_WIDTH, D_MODEL), CONV_WIDTH),
        "conv_w_out": w(ks[11], (D_MODEL, D_MODEL), D_MODEL),
        "norm_ffn1": gain(ks[12]),
        "moe_router": w(ks[13], (D_MODEL, N_EXPERTS), D_MODEL),
        "moe_w_gate": w(ks[14], (N_EXPERTS, D_MODEL, D_FF), D_MODEL),
        "moe_w_up": w(ks[15], (N_EXPERTS, D_MODEL, D_FF), D_MODEL),
        "moe_w_down": w(ks[16], (N_EXPERTS, D_FF, D_MODEL), D_FF),
        "norm_final": gain(ks[17]),
    }


def reference(x, norm_mix0, ret_w_in, ret_w_out, norm_ffn0, ffn_w_gate, ffn_w_up, ffn_w_down,
              norm_mix1, conv_w_in, conv_w, conv_w_out, norm_ffn1, moe_router, moe_w_gate,
              moe_w_up, moe_w_down, norm_final):
    mixer_params = [(norm_mix0, ret_w_in, ret_w_out), (norm_mix1, conv_w_in, conv_w, conv_w_out)]
    ffn_params = [(norm_ffn0, ffn_w_gate, ffn_w_up, ffn_w_down),
                  (norm_ffn1, moe_router, moe_w_gate, moe_w_up, moe_w_down)]
    h = x
    for i in range(DEPTH):
        mp = mixer_params[i]
        if i % 2 == 0:
            h = h + _retention(_rmsnorm(h, mp[0]), mp[1], mp[2])
        else:
            h = h + _short_conv(_rmsnorm(h, mp[0]), mp[1], mp[2], mp[3])
        fp = ffn_params[i]
        if i % 2 == 0:
            h = h + _swiglu(_rmsnorm(h, fp[0]), fp[1], fp[2], fp[3])
        else:
            h = h + _moe_swiglu(_rmsnorm(h, fp[0]), fp[1], fp[2], fp[3], fp[4])
    return _rmsnorm(h, norm_final)
```

```python
import contextlib
import numpy as np
import concourse.bass as bass
import concourse.mybir as mybir
from concourse.bass_utils import run_bass_kernel_spmd

F32 = mybir.dt.float32
BF16 = mybir.dt.bfloat16
AF = mybir.ActivationFunctionType
ALU = mybir.AluOpType
AX = mybir.AxisListType

ENGS = ("sp", "act", "dve", "pool", "pe")


class Buf:
    __slots__ = ("name", "ap", "writers", "readers", "disjoint")

    def __init__(self, name, ap=None, disjoint=False):
        self.name = name
        self.ap = ap
        self.disjoint = disjoint
        self.writers = {}
        self.readers = {}


class Op:
    __slots__ = ("emit", "deps", "marked", "dma")

    def __init__(self, emit, deps, dma):
        self.emit = emit
        self.deps = deps
        self.marked = False
        self.dma = dma


class FW:
    def __init__(self, nc):
        self.nc = nc
        self.ops = {e: [] for e in ENGS}
        self.dma_count = {}
        self.n_waits = 0

    def _collect(self, eng, reads, writes):
        deps = {}

        def add(tok):
            cls = (tok[0], tok[1])
            if deps.get(cls, -1) < tok[2]:
                deps[cls] = tok[2]

        for b in reads:
            for tok in b.writers.values():
                if tok[0] == "eng" and tok[1] == "pe" and eng == "pe":
                    continue
                add(tok)
        for b in writes:
            for d in (b.writers, b.readers):
                for tok in d.values():
                    if tok[0] == "eng" and tok[1] == eng:
                        continue
                    if d is b.writers and b.disjoint and tok[0] == "dma" and eng.startswith("dma:"):
                        continue
                    add(tok)
        return deps

    def _mark(self, deps):
        for (kind, k), v in deps.items():
            if kind == "eng":
                self.ops[k][v].marked = True

    def op(self, eng, emit, reads=(), writes=()):
        deps = self._collect(eng, reads, writes)
        self._mark(deps)
        idx = len(self.ops[eng])
        self.ops[eng].append(Op(emit, deps, None))
        tok = ("eng", eng, idx)
        for b in reads:
            b.readers[eng] = tok
        for b in writes:
            b.writers[eng] = tok

    def dma(self, eng, key, emit, reads=(), writes=()):
        cls = "dma:" + key
        deps = self._collect(cls, reads, writes)
        self._mark(deps)
        val = self.dma_count.get(key, 0) + 16
        self.dma_count[key] = val
        self.ops[eng].append(Op(emit, deps, (key, val)))
        tok = ("dma", key, val)
        for b in reads:
            b.readers[cls] = tok
        for b in writes:
            b.writers[cls] = tok

    def fence(self, eng, bufs):
        deps = self._collect(eng + "_fence", list(bufs), list(bufs))
        self._mark(deps)
        self.ops[eng].append(Op(None, deps, None))

    def emit_all(self, stack):
        nc = self.nc
        sems = {e: stack.enter_context(nc.semaphore("s_" + e)) for e in ENGS if e != "sp"}
        dsems = {k: stack.enter_context(nc.semaphore("d_" + k)) for k in self.dma_count}
        counts = {}
        for e in ENGS:
            c = 0
            arr = []
            for o in self.ops[e]:
                if o.marked:
                    c += 1
                arr.append(c)
            counts[e] = arr
        block = stack.enter_context(nc.Block())
        fw = self

        def run(e, engobj):
            water = {}
            for o in fw.ops[e]:
                for (kind, k), v in o.deps.items():
                    if kind == "eng":
                        sem = sems[k]
                        val = counts[k][v]
                    else:
                        sem = dsems[k]
                        val = v
                    wk = (kind, k)
                    if water.get(wk, 0) < val:
                        engobj.wait_ge(sem, val)
                        water[wk] = val
                        fw.n_waits += 1
                if o.emit is None:
                    continue
                inst = o.emit(engobj)
                if o.dma is not None:
                    inst.then_inc(dsems[o.dma[0]], 16)
                elif o.marked:
                    inst.then_inc(sems[e], 1)

        @block.sync
        def _(eng):
            run("sp", eng)

        @block.scalar
        def _(eng):
            run("act", eng)

        @block.vector
        def _(eng):
            run("dve", eng)

        @block.gpsimd
        def _(eng):
            run("pool", eng)

        @block.tensor
        def _(eng):
            run("pe", eng)


I32 = mybir.dt.int32
D = 1024
H = 4
FF = 3584
NE = 8
TT = 512
EPS = 1e-6
NW = 4
LA = NW - 2
GAM = [1.0 - 2.0 ** (-5.0 - h) for h in range(H)]


def build_nc(S, dbg=False):
    NT = S // TT
    NB = NT * 4
    NSLOT = (2 * S) // 512 + NE
    nc = bass.Bass("TRN2", target_bir_lowering=False)

    def din(name, shape, dt=F32):
        return nc.dram_tensor(name, list(shape), dt, kind="ExternalInput").ap()

    x = din("x", [S, D])
    ret_w_in = din("ret_w_in", [D, 6144])
    ret_w_out = din("ret_w_out", [2048, D])
    ffn_w_gate = din("ffn_w_gate", [D, FF])
    ffn_w_up = din("ffn_w_up", [D, FF])
    ffn_w_down = din("ffn_w_down", [FF, D])
    conv_w_in = din("conv_w_in", [D, 3072])
    conv_w_out = din("conv_w_out", [D, D])
    moe_w_gate = din("moe_w_gate", [NE, D, FF])
    moe_w_up = din("moe_w_up", [NE, D, FF])
    moe_w_down = din("moe_w_down", [NE, FF, D])
    small_d = din("small", [128, 128])
    consts_d = din("consts", [128, 520])
    tabs_d = din("tabs", [4, 128, S])
    gfin_d = din("gfin", [128, D])
    out = nc.dram_tensor("out", [S, D], F32, kind="ExternalOutput").ap()
    if dbg:
        dbg_d = nc.dram_tensor("dbg", [4, 8, 128, S], F32, kind="ExternalOutput").ap()

    blocks = []

    def addA(w, c0):
        blocks.append(("A", w, c0))
        return len(blocks) - 1

    def addB(w, c0):
        blocks.append(("B", w, c0))
        return len(blocks) - 1

    def addC(w, c0):
        blocks.append(("C", w, c0))
        return len(blocks) - 1

    bl_qk = [addA(ret_w_in, c * 512) for c in range(4)]
    bl_v = [addA(ret_w_in, 2048 + c * 512) for c in range(4)]
    bl_g = [addA(ret_w_in, 4096 + c * 512) for c in range(4)]
    bl_wo = [addB(ret_w_out, c * 256) for c in range(4)]
    bl_ffn = []
    for fb in range(7):
        bl_ffn.append(addA(ffn_w_gate, fb * 512))
        bl_ffn.append(addA(ffn_w_up, fb * 512))
    for dc in range(8):
        bl_ffn.append(addC(ffn_w_down, dc * 128))
    bl_conv = []
    for half in range(2):
        bl_conv.append((addA(conv_w_in, 1024 + half * 512), addA(conv_w_in, 2048 + half * 512),
                        addA(conv_w_in, half * 512)))
    bl_co = [addA(conv_w_out, c * 512) for c in range(2)]
    NBLK1 = len(blocks)
    moe_blocks = []
    for e in range(NE):
        for fb in range(7):
            moe_blocks.append(("A", moe_w_gate[e], fb * 512))
            moe_blocks.append(("A", moe_w_up[e], fb * 512))
        for dc in range(8):
            moe_blocks.append(("C", moe_w_down[e], dc * 128))
    NROWS = (NBLK1 + len(moe_blocks)) * 128
    wscr = nc.dram_tensor("wscr", [NROWS, 4096], BF16, kind="Internal").ap()
    h3_scr = nc.dram_tensor("h3_scr", [S, D], F32, kind="Internal").ap()
    xn_scr = nc.dram_tensor("xn_scr", [S, D], BF16, kind="Internal").ap()
    xg = nc.dram_tensor("xg", [NSLOT * 512, D], BF16, kind="Internal").ap()
    yg = nc.dram_tensor("yg", [NSLOT * 512, D], F32, kind="Internal").ap()
    scrb = [Buf(f"scr{i}") for i in range(NBLK1)]
    moescrb = Buf("moescr")
    h3b = Buf("h3scr")
    xnsb = Buf("xnscr")
    xgz = Buf("xgz", disjoint=True)
    xgs = Buf("xgs", disjoint=True)
    ygb = Buf("yg")
    outb = Buf("outdram", disjoint=True)

    st = contextlib.ExitStack()
    with st:
        fw = FW(nc)
        ARENA = 47800
        arena = st.enter_context(nc.sbuf_tensor("arena", [128, ARENA], F32))
        off = [0]

        def carve(n):
            a = arena[:, off[0]:off[0] + n]
            off[0] += n
            assert off[0] <= ARENA, off[0]
            return a

        def v3(ap, b):
            return ap.rearrange("p (a b) -> p a b", b=b)

        hT_all = carve(4096)
        hT = v3(hT_all, 512)
        hTb = [Buf(f"hT{k}") for k in range(8)]
        xnT_all = carve(2048).bitcast(BF16)
        xnT = v3(xnT_all, 512)
        xnTb = [Buf(f"xnT{k}") for k in range(8)]
        io_ap = [carve(1024), carve(1024)]
        iob = [Buf("io0"), Buf("io1")]
        tab_all = carve(2048)
        tab = v3(tab_all, 512)
        tabb = Buf("tab")
        qk_f = carve(4096)
        xin = v3(qk_f, 1024)
        qk_all = qk_f.bitcast(BF16)
        qT = v3(qk_all[:, 0:4096], 512)
        kT = v3(qk_all[:, 4096:8192], 512)
        ogT = v3(qk_all, 512)
        yT = qT
        QA = Buf("QA")
        QB = Buf("QB")
        ks_all = carve(2048).bitcast(BF16)
        ktok = v3(ks_all, 1024)
        sq_all = ks_all
        sq = v3(ks_all, 512)
        xs3 = ktok
        KS = Buf("KS")
        va_f = carve(4224)
        VA = Buf("VA")
        vtok = v3(va_f[:, 0:4096].bitcast(BF16), 2048)
        uT = v3(va_f[:, 0:4112], 514)
        xnf = uT[:, :, 2:514]
        hffA = v3(va_f[:, 0:4096].bitcast(BF16), 512)
        vb_f = carve(4096)
        VB = Buf("VB")
        gtok = v3(vb_f.bitcast(BF16), 2048)
        hffB = v3(vb_f.bitcast(BF16), 512)
        stF = [v3(carve(1024), 512) for _ in range(H)]
        stFb = [[Buf(f"stF{h}_{dc}") for dc in range(2)] for h in range(H)]
        stB = [v3(carve(512).bitcast(BF16), 512) for _ in range(H)]
        stBb = [[Buf(f"stB{h}_{dc}") for dc in range(2)] for h in range(H)]
        halo = v3(carve(16), 2)
        halob = Buf("halo")
        wsl_off = off[0]
        wsl = [carve(2048).bitcast(BF16) for _ in range(NW)]
        wslb = [Buf(f"ws{i}") for i in range(NW)]
        NTMP = 6
        tmp_all = carve(512 * NTMP)
        tmp_ap = [tmp_all[:, i * 512:(i + 1) * 512] for i in range(NTMP)]
        tmpb = [Buf(f"tmp{i}") for i in range(NTMP)]
        rstd = carve(512)
        rstdb = Buf("rstd")
        consts = carve(520)
        constb = Buf("consts")
        small = carve(128)
        smallb = Buf("small")
        identf = carve(128)
        identfb = Buf("identf")
        iot = carve(128)
        identb = carve(64).bitcast(BF16)
        identbb = Buf("identb")
        onesb = carve(64).bitcast(BF16)
        onesbb = Buf("onesb")
        lstrb = carve(64).bitcast(BF16)
        lstrbb = Buf("lstr")
        epsb = carve(1)
        epsbb = Buf("eps")
        sT_ap = [carve(256).bitcast(BF16), carve(256).bitcast(BF16)]
        sTb = [Buf("sT0"), Buf("sT1")]
        ssq4 = carve(16)
        ssqhb = [Buf(f"ssqh{h}") for h in range(H)]
        junk = carve(256).bitcast(BF16)
        junkb = Buf("junk")
        ssq = carve(4)
        ssqb = Buf("ssq")
        lg = carve(32)
        lgb = Buf("lg")
        m8 = carve(32)
        m8b = Buf("m8")
        rt = carve(64)
        rtb = Buf("rt")
        gA = carve(NB * 8)
        gAb = Buf("gA")
        s1A = carve(NB * 8)
        s1b = Buf("s1A")
        p2s = carve(256)
        P2 = Buf("P2")
        zt = carve(512).bitcast(BF16)
        ztb = Buf("zt")

        PS = []
        for i in range(8):
            PS.append((st.enter_context(nc.psum_tensor(f"ps{i}", [128, 512], F32))[:, :], Buf(f"ps{i}")))
        psi = [0]

        def nps():
            p = PS[psi[0] % 8]
            psi[0] += 1
            return p

        tmi = [0]

        def ntmp():
            i = tmi[0] % NTMP
            tmi[0] += 1
            return tmp_ap[i], tmpb[i], i

        Mt = consts[:, 0:512]
        qdec = consts[:, 512:516]
        kdec = consts[:, 516:520]

        def mm(psap, psbuf, lhsT, rhs, start, stop, reads):
            fw.op("pe", lambda e: e.matmul(psap, lhsT=lhsT, rhs=rhs, start=start, stop=stop),
                  reads=reads, writes=[psbuf])

        def tr(psap, psbuf, in_, ident, reads):
            fw.op("pe", lambda e: e.transpose(psap, in_=in_, identity=ident), reads=reads, writes=[psbuf])

        def act(out_, in_, func, reads, writes, scale=None, bias=None, accum=None):
            kw = {}
            if scale is not None:
                kw["scale"] = scale
            if bias is not None:
                kw["bias"] = bias
            if accum is not None:
                kw["accum_out"] = accum
            fw.op("act", lambda e: e.activation(out=out_, in_=in_, func=func, **kw), reads=reads, writes=writes)

        def tt(eng, out_, in0, in1, op, reads, writes):
            fw.op(eng, lambda e: e.tensor_tensor(out=out_, in0=in0, in1=in1, op=op), reads=reads, writes=writes)

        def stt(out_, in0, scalar, in1, op0, op1, reads, writes):
            fw.op("dve", lambda e: e.scalar_tensor_tensor(out=out_, in0=in0, scalar=scalar, in1=in1, op0=op0, op1=op1),
                  reads=reads, writes=writes)

        def ts(eng, out_, in0, s1, op0, reads, writes, s2=None, op1=None):
            if op1 is None:
                fw.op(eng, lambda e: e.tensor_scalar(out=out_, in0=in0, scalar1=s1, scalar2=None, op0=op0),
                      reads=reads, writes=writes)
            else:
                fw.op(eng, lambda e: e.tensor_scalar(out=out_, in0=in0, scalar1=s1, scalar2=s2, op0=op0, op1=op1),
                      reads=reads, writes=writes)

        def cp(eng, out_, in_, reads, writes):
            fw.op(eng, lambda e: e.tensor_copy(out=out_, in_=in_), reads=reads, writes=writes)

        def red(out_, in_, reads, writes):
            fw.op("dve", lambda e: e.tensor_reduce(out=out_, in_=in_, axis=AX.X, op=ALU.add), reads=reads, writes=writes)

        def dmas(eng, key, out_, in_, reads, writes):
            fw.dma(eng, key, lambda e: e.dma_start(out=out_, in_=in_), reads=reads, writes=writes)

        dmas("sp", "cst", consts, consts_d[:, :], [], [constb])
        dmas("sp", "cst2", small, small_d[:, :], [], [smallb])
        iotb = Buf("iot")
        fw.op("pool", lambda e: e.iota(iot, pattern=[[1, 128]], base=0, channel_multiplier=-1,
                                       allow_small_or_imprecise_dtypes=True), writes=[iotb])
        fw.op("dve", lambda e: e.tensor_single_scalar(out=identf, in_=iot, scalar=0.0, op=ALU.is_equal),
              reads=[iotb], writes=[identfb])
        fw.op("dve", lambda e: e.tensor_single_scalar(out=identb, in_=iot, scalar=0.0, op=ALU.is_equal),
              reads=[iotb], writes=[identbb])
        fw.op("dve", lambda e: e.tensor_single_scalar(out=lstrb, in_=iot, scalar=0.0, op=ALU.is_gt),
              reads=[iotb], writes=[lstrbb])
        fw.op("dve", lambda e: e.memset(onesb, 1.0), writes=[onesbb])
        fw.op("dve", lambda e: e.memset(epsb, EPS), writes=[epsbb])
        for h in range(H):
            fw.op("pool", (lambda hh: lambda e: e.memset(stF[hh], 0.0))(h), writes=stFb[h])
            fw.op("pool", (lambda hh: lambda e: e.memset(stB[hh], 0.0))(h), writes=stBb[h])
        fw.op("pool", lambda e: e.memset(halo, 0.0), writes=[halob])
        fw.op("pool", lambda e: e.memset(zt, 0.0), writes=[ztb])

        seq = [("R", t, bi) for t in range(NT) for bi in range(NBLK1)]
        N1 = len(seq)
        seq += [("M", s, j) for s in range(NSLOT) for j in range(22)]
        issued = [0]
        barrier = [N1]
        widx_ref = [None, None]

        def blk_n(kind):
            return 4096 if kind in ("A", "B") else 3584

        def src_view(kind, w, c0):
            if kind == "A":
                return w[:, c0:c0 + 512].rearrange("(k p) j -> p k j", p=128), 512
            if kind == "B":
                return w[:, c0:c0 + 256].rearrange("(k p) j -> p k j", p=128), 256
            return w[:, c0:c0 + 128].rearrange("(k p) j -> p k j", p=128), 128

        def issue_load(g):
            ent = seq[g]
            si = g % NW
            slot = wsl[si]
            sb = wslb[si]
            if ent[0] == "M":
                _, s, j = ent
                col = s * 22 + j
                widx, widxb = widx_ref
                fw.dma("pool", f"w{si}", lambda e: e.indirect_dma_start(
                    out=slot, out_offset=None, in_=wscr[:, :],
                    in_offset=bass.IndirectOffsetOnAxis(ap=widx[:, col:col + 1], axis=0)), reads=[moescrb, widxb], writes=[sb])
                return
            _, t, bi = ent
            kind, w, c0 = blocks[bi]
            n = blk_n(kind)
            rows = wscr[bi * 128:(bi + 1) * 128, 0:n]
            if t == 0:
                src, jw = src_view(kind, w, c0)
                dst = v3(slot[:, 0:n], jw)
                if kind == "C":
                    dmas("pool", f"w{si}", dst[:, 0:14, :], src[:, 0:14, :], [], [sb])
                    dmas("pool", f"w{si}", dst[:, 14:28, :], src[:, 14:28, :], [], [sb])
                else:
                    dmas("pool", f"w{si}", dst, src, [], [sb])
                if NT > 1:
                    dmas("sp", f"s{si}", rows, slot[:, 0:n], [sb], [scrb[bi]])
            else:
                dmas("sp", f"w{si}", slot[:, 0:n], rows, [scrb[bi]], [sb])

        gctr = [0]

        def fetch(key):
            g = gctr[0]
            assert seq[g] == key, (seq[g], key)
            gctr[0] += 1
            while issued[0] <= min(g + LA, barrier[0] - 1):
                issue_load(issued[0])
                issued[0] += 1
            si = g % NW
            return wsl[si], wslb[si]

        cv_next = [0]
        cv_per_fetch = -(-len(moe_blocks) // max(1, (NT - 1) * NBLK1))
        cv_stride = max(1, ((NT - 1) * NBLK1) // len(moe_blocks))
        cv_tick = [0]

        def cv_some(n):
            for _ in range(n):
                i = cv_next[0]
                if i >= len(moe_blocks):
                    return
                cv_next[0] += 1
                kind, w, c0 = moe_blocks[i]
                src, jw = src_view(kind, w, c0)
                nn = blk_n(kind)
                r0 = (NBLK1 + i) * 128
                dst = wscr[r0:r0 + 128, 0:nn].rearrange("p (k j) -> p k j", j=jw)
                if kind == "C":
                    dmas("pool", "cv", dst[:, 0:14, :], src[:, 0:14, :], [], [moescrb])
                    dmas("pool", "cv", dst[:, 14:28, :], src[:, 14:28, :], [], [moescrb])
                else:
                    dmas("pool", "cv", dst, src, [], [moescrb])

        zf_next = [0]

        def zf_some(n):
            for _ in range(n):
                r = zf_next[0]
                if r >= NSLOT * 4:
                    return
                zf_next[0] += 1
                dmas("sp", "zf", xg[r * 128:(r + 1) * 128, :], zt, [ztb], [xgz])

        def fetch1(t, bi):
            if t >= 1 or NT == 1:
                cv_tick[0] += 1
                if cv_tick[0] % cv_stride == 0:
                    cv_some(cv_per_fetch)
                zf_some(1 if NT > 4 else 8)
            return fetch(("R", t, bi))

        def hff(f):
            return (hffA[:, f, :], VA) if f < 16 else (hffB[:, f - 16, :], VB)

        def sq_chunk(k):
            act(sq[:, k, :], hT[:, k, :], AF.Square, reads=[hTb[k]], writes=[KS])

        def rmsnorm(nidx, f32out=False, presq=True):
            if not presq:
                act(sq_all, hT_all, AF.Square, reads=hTb, writes=[KS])
            pa, pb = nps()
            for k in range(8):
                mm(pa, pb, onesb, sq[:, k, :], k == 0, k == 7, [onesbb, KS])
            act(rstd, pa, AF.Ln, reads=[pb, epsbb], writes=[rstdb], scale=1.0 / D, bias=epsb[:, 0:1])
            act(rstd, rstd, AF.Exp, reads=[rstdb], writes=[rstdb], scale=-0.5)
            for k in range(8):
                g = small[:, nidx * 8 + k:nidx * 8 + k + 1]
                if f32out:
                    stt(xnf[:, k, :], hT[:, k, :], g, rstd, ALU.mult, ALU.mult, [hTb[k], smallb, rstdb], [VA])
                else:
                    stt(xnT[:, k, :], hT[:, k, :], g, rstd, ALU.mult, ALU.mult, [hTb[k], smallb, rstdb], [xnTb[k]])

        def dump(stage, t):
            if dbg:
                dst = dbg_d[stage, :, :, t * TT:(t + 1) * TT].rearrange("k p s -> p k s")
                dmas("pool", "dbg", dst, hT, hTb, [])

        def resid_add(dchunk, pa, pb):
            tt("dve", hT[:, dchunk, :], pa, hT[:, dchunk, :], ALU.add, [pb, hTb[dchunk]], [hTb[dchunk]])
            sq_chunk(dchunk)

        def ffn(fetchj, evac):
            for fb in range(7):
                sg, sgb = fetchj(2 * fb)
                su, sub = fetchj(2 * fb + 1)
                sg3 = v3(sg, 512)
                su3 = v3(su, 512)
                for fc in range(4):
                    f = fb * 4 + fc
                    pga, pgb = nps()
                    pua, pub = nps()
                    for k in range(8):
                        mm(pga, pgb, sg3[:, k, fc * 128:(fc + 1) * 128], xnT[:, k, :], k == 0, k == 7, [sgb, xnTb[k]])
                    for k in range(8):
                        mm(pua, pub, su3[:, k, fc * 128:(fc + 1) * 128], xnT[:, k, :], k == 0, k == 7, [sub, xnTb[k]])
                    ta, tb_, _ = ntmp()
                    act(ta, pga, AF.Silu, reads=[pgb], writes=[tb_])
                    ha, hb = hff(f)
                    tt("dve", ha, pua, ta, ALU.mult, [pub, tb_], [hb])
            for dc in range(8):
                sd, sdb = fetchj(14 + dc)
                sd3 = v3(sd[:, 0:3584], 128)
                pa, pb = nps()
                for f in range(28):
                    ha, hb = hff(f)
                    mm(pa, pb, sd3[:, f, :], ha, f == 0, f == 27, [sdb, hb])
                evac(dc, pa, pb)

        def store_tokmajor_f32(srcT, src_reads, dram_rows, dram_buf, queue):
            for tb in range(4):
                io = io_ap[tb % 2]
                ib = iob[tb % 2]
                for half in range(2):
                    pa, pb = nps()
                    for kk in range(4):
                        k = half * 4 + kk
                        tr(pa[:, kk * 128:(kk + 1) * 128], pb, srcT[:, k, tb * 128:(tb + 1) * 128], identf,
                           src_reads(k) + [identfb])
                    if half == 0:
                        act(io[:, 0:512], pa, AF.Copy, reads=[pb], writes=[ib])
                    else:
                        cp("dve", io[:, 512:1024], pa, [pb], [ib])
                dmas(queue, f"io{tb % 2}", dram_rows(tb), io, [ib], [dram_buf])

        def load_x(t_, half):
            r0 = t_ * TT + half * 256
            src = x[r0:r0 + 256, :].rearrange("(tb p) d -> p tb d", p=128)
            dmas("sp", f"xin{half}", xin[:, half * 2:half * 2 + 2, :], src, [], [QA if half == 0 else QB])

        for t in range(NT):
            tok0 = t * TT
            tsrc = tabs_d[:, :, tok0:tok0 + TT].rearrange("a p s -> p a s")
            dmas("sp", "tab", tab, tsrc, [], [tabb])
            if t == 0:
                load_x(0, 1)
                load_x(0, 0)
            for tb in (2, 3, 0, 1):
                xb_ = QA if tb < 2 else QB
                for half in range(2):
                    pa, pb = nps()
                    for kk in range(4):
                        k = half * 4 + kk
                        tr(pa[:, kk * 128:(kk + 1) * 128], pb, xin[:, tb, k * 128:(k + 1) * 128], identf, [xb_, identfb])
                    dst = hT[:, half * 4:(half + 1) * 4, tb * 128:(tb + 1) * 128]
                    srcv = v3(pa, 128)
                    wr = [hTb[half * 4 + kk] for kk in range(4)]
                    if half == 0:
                        act(dst, srcv, AF.Copy, reads=[pb], writes=wr)
                    else:
                        cp("dve", dst, srcv, [pb], wr)
                act(sq[:, :, tb * 128:(tb + 1) * 128], hT[:, :, tb * 128:(tb + 1) * 128], AF.Square, reads=hTb, writes=[KS])
            dump(0, t)
            rmsnorm(0)
            for blk in range(4):
                sl, slb = fetch1(t, bl_qk[blk])
                sl3 = v3(sl, 512)
                isq = blk < 2
                for hh in range(2):
                    h = (blk % 2) * 2 + hh
                    p1a, p1b = nps()
                    p2a, p2b = nps()
                    for k in range(8):
                        mm(p1a, p1b, sl3[:, k, (hh * 2) * 128:(hh * 2 + 1) * 128], xnT[:, k, :], k == 0, k == 7, [slb, xnTb[k]])
                    for k in range(8):
                        mm(p2a, p2b, sl3[:, k, (hh * 2 + 1) * 128:(hh * 2 + 2) * 128], xnT[:, k, :], k == 0, k == 7, [slb, xnTb[k]])
                    cos = tab[:, 0 if isq else 2, :]
                    sin = tab[:, 1 if isq else 3, :]
                    dstT = qT if isq else kT
                    dbuf = QA if isq else QB
                    aa, ab, _ = ntmp()
                    ba, bb, _ = ntmp()
                    ca, cb, _ = ntmp()
                    da, db, _ = ntmp()
                    tt("dve", aa, p1a, cos, ALU.mult, [p1b, tabb], [ab])
                    tt("dve", ba, p2a, sin, ALU.mult, [p2b, tabb], [bb])
                    tt("dve", ca, p1a, sin, ALU.mult, [p1b, tabb], [cb])
                    tt("dve", da, p2a, cos, ALU.mult, [p2b, tabb], [db])
                    tt("dve", dstT[:, h * 2, :], aa, ba, ALU.subtract, [ab, bb], [dbuf])
                    tt("dve", dstT[:, h * 2 + 1, :], ca, da, ALU.add, [cb, db], [dbuf])
            for h in range(H):
                sl, slb = fetch1(t, bl_v[h])
                sl3 = v3(sl, 512)
                for tb in range(4):
                    pa, pb = nps()
                    for k in range(8):
                        mm(pa, pb, xnT[:, k, tb * 128:(tb + 1) * 128], sl3[:, k, :], k == 0, k == 7, [slb, xnTb[k]])
                    cp("dve", vtok[:, tb, h * 512:(h + 1) * 512], pa, [pb], [VA])
            for h in range(H):
                sl, slb = fetch1(t, bl_g[h])
                sl3 = v3(sl, 512)
                for tb in range(4):
                    pa, pb = nps()
                    for k in range(8):
                        mm(pa, pb, xnT[:, k, tb * 128:(tb + 1) * 128], sl3[:, k, :], k == 0, k == 7, [slb, xnTb[k]])
                    act(gtok[:, tb, h * 512:(h + 1) * 512], pa, AF.Silu, reads=[pb], writes=[VB])
            for tb in range(4):
                pa, pb = nps()
                pab = pa.bitcast(BF16)
                for c8 in range(8):
                    tr(pab[:, c8 * 128:(c8 + 1) * 128], pb, kT[:, c8, tb * 128:(tb + 1) * 128], identb, [QB, identbb])
                for h in range(H):
                    act(ktok[:, tb, h * 256:(h + 1) * 256], pab[:, h * 256:(h + 1) * 256], AF.Copy,
                        reads=[pb, constb], writes=[KS], scale=kdec[:, h:h + 1])
            def stageA(c):
                cs = slice(c * 128, (c + 1) * 128)
                psa, psb_ = nps()
                for h in range(H):
                    for dc in range(2):
                        mm(psa[:, h * 128:(h + 1) * 128], psb_, kT[:, h * 2 + dc, cs], qT[:, h * 2 + dc, cs], dc == 0, dc == 1, [QA, QB])
                tt("dve", sT_ap[c % 2], psa, Mt, ALU.mult, [psb_, constb], [sTb[c % 2]])

            def stageB(c):
                cs = slice(c * 128, (c + 1) * 128)
                for h in range(H):
                    hs = slice(h * 512, (h + 1) * 512)
                    sq_ = ssq4[:, h * 4:(h + 1) * 4]
                    poa, pob = nps()
                    mm(poa, pob, sT_ap[c % 2][:, h * 128:(h + 1) * 128], vtok[:, c, hs], True, False, [sTb[c % 2], VA])
                    for dc in range(2):
                        mm(poa, pob, qT[:, h * 2 + dc, cs], stB[h][:, dc, :], False, dc == 1, [QA, stBb[h][dc]])
                    pts = []
                    for dc in range(2):
                        pta, ptb = nps()
                        mm(pta, ptb, ktok[:, c, h * 256 + dc * 128:h * 256 + (dc + 1) * 128], vtok[:, c, hs], True, True, [KS, VA])
                        pts.append((pta, ptb))
                    act(junk, poa, AF.Square, reads=[pob, constb], writes=[junkb, ssqhb[h]], scale=qdec[:, h:h + 1], accum=sq_[:, 0:1])
                    act(sq_[:, 1:2], sq_[:, 0:1], AF.Sqrt, reads=[ssqhb[h], epsbb], writes=[ssqhb[h]], scale=1.0 / 512, bias=epsb[:, 0:1])
                    for dc in range(2):
                        pta, ptb = pts[dc]
                        stt(stF[h][:, dc, :], stF[h][:, dc, :], float(GAM[h] ** 128), pta, ALU.mult, ALU.add,
                            [stFb[h][dc], ptb], [stFb[h][dc]])
                        act(stB[h][:, dc, :], stF[h][:, dc, :], AF.Copy, reads=[stFb[h][dc]], writes=[stBb[h][dc]])
                    fw.op("dve", (lambda q_: lambda e: e.reciprocal(out=q_[:, 2:3], in_=q_[:, 1:2]))(sq_), reads=[ssqhb[h]], writes=[ssqhb[h]])
                    tt("dve", sq_[:, 3:4], sq_[:, 2:3], qdec[:, h:h + 1], ALU.mult, [ssqhb[h], constb], [ssqhb[h]])
                    stt(gtok[:, c, hs], poa, sq_[:, 3:4], gtok[:, c, hs], ALU.mult, ALU.mult, [pob, ssqhb[h], VB], [VB])

            for c in range(4):
                stageA(c)
                stageB(c)
            for ec2 in range(8):
                pa, pb = nps()
                pab = pa.bitcast(BF16)
                for e2 in range(2):
                    ec = ec2 * 2 + e2
                    for tb in range(4):
                        tr(pab[:, e2 * 512 + tb * 128:e2 * 512 + (tb + 1) * 128], pb, gtok[:, tb, ec * 128:(ec + 1) * 128],
                           identb, [VB, identbb])
                dst = ogT[:, ec2 * 2:ec2 * 2 + 2, :]
                srcv = v3(pab, 512)
                qb = QA if ec2 < 4 else QB
                if ec2 % 2 == 0:
                    act(dst, srcv, AF.Copy, reads=[pb], writes=[qb])
                else:
                    cp("dve", dst, srcv, [pb], [qb])
            for blk in range(4):
                sl, slb = fetch1(t, bl_wo[blk])
                sl3 = v3(sl, 256)
                for dd in range(2):
                    dch = blk * 2 + dd
                    pa, pb = nps()
                    for ek in range(16):
                        mm(pa, pb, sl3[:, ek, dd * 128:(dd + 1) * 128], ogT[:, ek, :], ek == 0, ek == 15,
                           [slb, QA if ek < 8 else QB])
                    resid_add(dch, pa, pb)
            if t + 1 < NT:
                load_x(t + 1, 1)
            dump(1, t)
            rmsnorm(1)
            ffn(lambda j: fetch1(t, bl_ffn[j]), resid_add)
            dump(2, t)
            rmsnorm(2)
            cp("dve", uT[:, :, 0:2], halo, [halob], [VA])
            for half in range(2):
                bc, bh, bb_ = bl_conv[half]
                sc, scb = fetch1(t, bc)
                sh, shb = fetch1(t, bh)
                sc3 = v3(sc, 512)
                sh3 = v3(sh, 512)
                for d4 in range(4):
                    dch = half * 4 + d4
                    pca, pcb = nps()
                    pha, phb = nps()
                    for k in range(8):
                        mm(pca, pcb, sc3[:, k, d4 * 128:(d4 + 1) * 128], xnT[:, k, :], k == 0, k == 7, [scb, xnTb[k]])
                    for k in range(8):
                        mm(pha, phb, sh3[:, k, d4 * 128:(d4 + 1) * 128], xnT[:, k, :], k == 0, k == 7, [shb, xnTb[k]])
                    ta, tb_, _ = ntmp()
                    act(ta, pca, AF.Copy, reads=[pcb], writes=[tb_])
                    tt("dve", uT[:, dch, 2:514], pha, ta, ALU.mult, [phb, tb_], [VA])
                sb_, sbb = fetch1(t, bb_)
                sb3 = v3(sb_, 512)
                for d4 in range(4):
                    dch = half * 4 + d4
                    pba, pbb = nps()
                    for k in range(8):
                        mm(pba, pbb, sb3[:, k, d4 * 128:(d4 + 1) * 128], xnT[:, k, :], k == 0, k == 7, [sbb, xnTb[k]])
                    ya, yb, _ = ntmp()
                    ts("dve", ya, uT[:, dch, 0:512], small[:, 40 + dch:41 + dch], ALU.mult, [VA, smallb], [yb])
                    stt(ya, uT[:, dch, 1:513], small[:, 48 + dch:49 + dch], ya, ALU.mult, ALU.add, [VA, smallb, yb], [yb])
                    stt(ya, uT[:, dch, 2:514], small[:, 56 + dch:57 + dch], ya, ALU.mult, ALU.add, [VA, smallb, yb], [yb])
                    tt("dve", yT[:, dch, :], pba, ya, ALU.mult, [pbb, yb], [QA])
            cp("dve", halo, uT[:, :, 512:514], [VA], [halob])
            for blk in range(2):
                sl, slb = fetch1(t, bl_co[blk])
                sl3 = v3(sl, 512)
                for d4 in range(4):
                    dch = blk * 4 + d4
                    pa, pb = nps()
                    for k in range(8):
                        mm(pa, pb, sl3[:, k, d4 * 128:(d4 + 1) * 128], yT[:, k, :], k == 0, k == 7, [slb, QA])
                    resid_add(dch, pa, pb)
            dump(3, t)
            if t + 1 < NT:
                load_x(t + 1, 0)
            rmsnorm(3, f32out=True)
            for k in range(0, 8, 2):
                act(xnT[:, k:k + 2, :], xnf[:, k:k + 2, :], AF.Copy, reads=[VA], writes=[xnTb[k], xnTb[k + 1]])
            pa, pb = nps()
            for tb in range(4):
                for k in range(8):
                    mm(pa[:, tb * 8:(tb + 1) * 8], pb, xnf[:, k, tb * 128:(tb + 1) * 128], small[:, 64 + k * 8:72 + k * 8],
                       k == 0, k == 7, [VA, smallb])
            cp("dve", lg, pa[:, 0:32], [pb], [lgb])
            for tb in range(4):
                l = lg[:, tb * 8:(tb + 1) * 8]
                m = m8[:, tb * 8:(tb + 1) * 8]
                mask = rt[:, 0:8]
                negm = rt[:, 8:9]
                ex = rt[:, 16:24]
                em = rt[:, 24:32]
                den = rt[:, 32:33]
                gcol = (t * 4 + tb) * 8
                fw.op("dve", (lambda m_, l_: lambda e: e.max(out=m_, in_=l_))(m, l), reads=[lgb], writes=[m8b])
                ts("dve", mask, l, m[:, 1:2], ALU.is_ge, [lgb, m8b], [rtb])
                ts("dve", s1A[:, gcol:gcol + 8], l, m[:, 0:1], ALU.is_equal, [lgb, m8b], [s1b])
                ts("dve", negm, m[:, 0:1], -1.0, ALU.mult, [m8b], [rtb])
                act(ex, l, AF.Exp, reads=[lgb, rtb], writes=[rtb], bias=negm, scale=1.0)
                tt("dve", em, ex, mask, ALU.mult, [rtb], [rtb])
                red(den, em, [rtb], [rtb])
                fw.op("dve", (lambda d_: lambda e: e.reciprocal(out=d_, in_=d_))(den), reads=[rtb], writes=[rtb])
                ts("dve", gA[:, gcol:gcol + 8], em, den, ALU.mult, [rtb], [gAb])
            store_tokmajor_f32(hT, lambda k: [hTb[k]], lambda tb: h3_scr[tok0 + tb * 128:tok0 + (tb + 1) * 128, :], h3b, "sp")
            for tb in range(4):
                pa, pb = nps()
                pab = pa.bitcast(BF16)
                for k in range(8):
                    tr(pab[:, k * 128:(k + 1) * 128], pb, xnT[:, k, tb * 128:(tb + 1) * 128], identb, [xnTb[k], identbb])
                ta, tb_, ti = ntmp()
                tab16 = ta.bitcast(BF16)
                if tb % 2 == 0:
                    act(tab16, pab, AF.Copy, reads=[pb], writes=[tb_])
                else:
                    cp("dve", tab16, pab, [pb], [tb_])
                dmas("sp", f"tm{ti}", xn_scr[tok0 + tb * 128:tok0 + (tb + 1) * 128, :], tab16, [tb_], [xnsb])
        cv_some(len(moe_blocks))
        zf_some(NSLOT * 4)

        W8 = NB * 8
        P2W = [P2] + tmpb + [rstdb, junkb]
        Mf = tmp_ap[0][:, 0:W8]
        csA = tmp_ap[1][:, 0:W8]
        csB = tmp_ap[2][:, 0:W8]
        pos = tmp_ap[3][:, 0:W8]
        dstf = tmp_ap[4][:, 0:W8]
        prod = tmp_ap[5][:, 0:W8]
        m2f = rstd[:, 0:W8]
        Mb16 = junk[:, 0:W8]

        def p2op(fn, extra_r=(), extra_w=()):
            fw.op("dve", fn, reads=[P2, gAb, s1b] + list(extra_r), writes=P2W + list(extra_w))

        p2op(lambda e: e.tensor_single_scalar(out=Mf, in_=gA, scalar=0.0, op=ALU.is_gt))
        p2op(lambda e: e.tensor_copy(out=Mb16, in_=Mf))
        p1a, p1b = nps()
        p2a, p2b = nps()
        mm(p1a[:, 0:W8], p1b, lstrb, Mb16, True, True, [lstrbb, P2])
        mm(p2a[:, 0:W8], p2b, onesb, Mb16, True, True, [onesbb, P2])
        p2op(lambda e: e.tensor_copy(out=csA, in_=p2a[:, 0:W8]), extra_r=[p2b])
        src_, dst_ = csA, csB
        k = 1
        while k < NB:
            s3, d3 = v3(src_, 8), v3(dst_, 8)
            p2op((lambda d3_, s3_, k_: lambda e: e.tensor_tensor(out=d3_[:, k_:, :], in0=s3_[:, k_:, :], in1=s3_[:, :NB - k_, :], op=ALU.add))(d3, s3, k))
            p2op((lambda d3_, s3_, k_: lambda e: e.tensor_copy(out=d3_[:, :k_, :], in_=s3_[:, :k_, :]))(d3, s3, k))
            src_, dst_ = dst_, src_
            k *= 2
        incl = src_
        incl3 = v3(incl, 8)
        pos3 = v3(pos, 8)
        lp3 = v3(p1a[:, 0:W8], 8)
        p2op(lambda e: e.tensor_copy(out=pos3[:, 0:1, :], in_=lp3[:, 0:1, :]), extra_r=[p1b])
        if NB > 1:
            p2op(lambda e: e.tensor_tensor(out=pos3[:, 1:, :], in0=lp3[:, 1:, :], in1=incl3[:, :NB - 1, :], op=ALU.add), extra_r=[p1b])
        n8 = incl3[:, NB - 1, :]
        thr = p2s[:, 0:NT]
        ns8 = p2s[:, 32:40]
        bi8 = p2s[:, 40:48]
        ends8 = p2s[:, 48:56]
        base8 = p2s[:, 56:64]
        sthr = p2s[:, 64:64 + NSLOT]
        es = p2s[:, 112:112 + NSLOT]
        jthr = p2s[:, 160:182]
        pcol = p2s[:, 182:183]
        wb = p2s[:, 184:184 + NSLOT]
        fw.op("pool", lambda e: e.iota(thr, pattern=[[512, NT]], base=0, channel_multiplier=0,
                                       allow_small_or_imprecise_dtypes=True), writes=[P2])
        fw.op("pool", lambda e: e.iota(sthr, pattern=[[512, NSLOT]], base=0, channel_multiplier=0,
                                       allow_small_or_imprecise_dtypes=True), writes=[P2])
        fw.op("pool", lambda e: e.iota(jthr, pattern=[[128, 22]], base=0, channel_multiplier=0,
                                       allow_small_or_imprecise_dtypes=True), writes=[P2])
        fw.op("pool", lambda e: e.iota(pcol, pattern=[[0, 1]], base=NBLK1 * 128, channel_multiplier=1,
                                       allow_small_or_imprecise_dtypes=True), writes=[P2])
        cmp1 = v3(prod[:, 0:8 * NT], NT)
        p2op(lambda e: e.tensor_tensor(out=cmp1, in0=n8.unsqueeze(2).to_broadcast([128, 8, NT]),
                                       in1=thr.unsqueeze(1).to_broadcast([128, 8, NT]), op=ALU.is_gt))
        p2op(lambda e: e.tensor_reduce(out=ns8, in_=cmp1, axis=AX.X, op=ALU.add))
        p2op(lambda e: e.tensor_copy(out=bi8[:, 0:1], in_=ns8[:, 0:1]))
        for e_ in range(1, NE):
            p2op((lambda e_i: lambda e: e.tensor_tensor(out=bi8[:, e_i:e_i + 1], in0=bi8[:, e_i - 1:e_i], in1=ns8[:, e_i:e_i + 1], op=ALU.add))(e_))
        p2op(lambda e: e.tensor_scalar(out=ends8, in0=bi8, scalar1=512.0, scalar2=None, op0=ALU.mult))
        p2op(lambda e: e.scalar_tensor_tensor(out=base8, in0=ns8, scalar=-512.0, in1=ends8, op0=ALU.mult, op1=ALU.add))
        dst3 = v3(dstf, 8)
        p2op(lambda e: e.tensor_tensor(out=dst3, in0=pos3, in1=base8.unsqueeze(1).to_broadcast([128, NB, 8]), op=ALU.add))
        p2op(lambda e: e.tensor_tensor(out=m2f, in0=Mf, in1=s1A, op=ALU.subtract))
        tabi = tab_all.bitcast(I32)
        idx1i = tabi[:, 0:NB]
        idx2i = tabi[:, 64:64 + NB]
        g1 = tab_all[:, 128:128 + NB]
        g2 = tab_all[:, 192:192 + NB]
        widxi = tabi[:, 256:256 + NSLOT * 22]
        assert 256 + NSLOT * 22 <= 2048
        i1f = csA[:, 0:NB]
        i2f = csB[:, 0:NB]
        prod3 = v3(prod, 8)
        p2op(lambda e: e.tensor_tensor(out=prod, in0=s1A, in1=dstf, op=ALU.mult))
        p2op(lambda e: e.tensor_reduce(out=i1f, in_=prod3, axis=AX.X, op=ALU.add))
        p2op(lambda e: e.tensor_copy(out=idx1i, in_=i1f), extra_w=[tabb])
        p2op(lambda e: e.tensor_tensor(out=prod, in0=m2f, in1=dstf, op=ALU.mult))
        p2op(lambda e: e.tensor_reduce(out=i2f, in_=prod3, axis=AX.X, op=ALU.add))
        p2op(lambda e: e.tensor_copy(out=idx2i, in_=i2f), extra_w=[tabb])
        p2op(lambda e: e.tensor_tensor(out=prod, in0=s1A, in1=gA, op=ALU.mult))
        p2op(lambda e: e.tensor_reduce(out=g1, in_=prod3, axis=AX.X, op=ALU.add), extra_w=[tabb])
        p2op(lambda e: e.tensor_tensor(out=prod, in0=m2f, in1=gA, op=ALU.mult))
        p2op(lambda e: e.tensor_reduce(out=g2, in_=prod3, axis=AX.X, op=ALU.add), extra_w=[tabb])
        cmp2 = v3(Mf[:, 0:NSLOT * 8] if NSLOT * 8 <= W8 else tmp_all[:, 0:NSLOT * 8], 8)
        p2op(lambda e: e.tensor_tensor(out=cmp2, in0=ends8.unsqueeze(1).to_broadcast([128, NSLOT, 8]),
                                       in1=sthr.unsqueeze(2).to_broadcast([128, NSLOT, 8]), op=ALU.is_le))
        p2op(lambda e: e.tensor_reduce(out=es, in_=cmp2, axis=AX.X, op=ALU.add))
        p2op(lambda e: e.tensor_scalar(out=es, in0=es, scalar1=float(NE - 1), scalar2=None, op0=ALU.min))
        p2op(lambda e: e.tensor_scalar(out=wb, in0=es, scalar1=float(22 * 128), scalar2=pcol, op0=ALU.mult, op1=ALU.add))
        widxf = v3(tmp_all[:, 1024:1024 + NSLOT * 22], 22)
        p2op(lambda e: e.tensor_tensor(out=widxf, in0=wb.unsqueeze(2).to_broadcast([128, NSLOT, 22]),
                                       in1=jthr.unsqueeze(1).to_broadcast([128, NSLOT, 22]), op=ALU.add))
        p2op(lambda e: e.tensor_copy(out=widxi, in_=tmp_all[:, 1024:1024 + NSLOT * 22]), extra_w=[tabb])
        widx_ref[0] = widxi
        widx_ref[1] = tabb
        barrier[0] = len(seq)

        for bg in range(NB):
            ta, tb_, ti = ntmp()
            stg = ta.bitcast(BF16)
            dmas("sp", f"tm{ti}", stg, xn_scr[bg * 128:(bg + 1) * 128, :], [xnsb], [tb_])
            for idx in (idx1i, idx2i):
                fw.dma("pool", f"sc{ti}", (lambda i_, b_, s_: lambda e: e.indirect_dma_start(
                    out=xg[:, :], out_offset=bass.IndirectOffsetOnAxis(ap=i_[:, b_:b_ + 1], axis=0),
                    in_=s_, in_offset=None))(idx, bg, stg),
                    reads=[tb_, tabb, xgz], writes=[xgs])

        def evac_y(dc, pa, pb):
            if dc % 2 == 0:
                act(hT[:, dc, :], pa, AF.Copy, reads=[pb], writes=[hTb[dc]])
            else:
                cp("dve", hT[:, dc, :], pa, [pb], [hTb[dc]])

        def load_xs(s_):
            dmas("sp", "xs", xs3, xg[s_ * 512:(s_ + 1) * 512, :].rearrange("(tb p) d -> p tb d", p=128), [xgz, xgs], [KS])

        load_xs(0)
        for s in range(NSLOT):
            for kp in range(4):
                pa, pb = nps()
                pab = pa.bitcast(BF16)
                for k2 in range(2):
                    k = kp * 2 + k2
                    for tb in range(4):
                        tr(pab[:, k2 * 512 + tb * 128:k2 * 512 + (tb + 1) * 128], pb, xs3[:, tb, k * 128:(k + 1) * 128],
                           identb, [KS, identbb])
                if kp % 2 == 0:
                    act(xnT[:, kp * 2:kp * 2 + 2, :], v3(pab, 512), AF.Copy, reads=[pb], writes=[xnTb[kp * 2], xnTb[kp * 2 + 1]])
                else:
                    cp("dve", xnT[:, kp * 2:kp * 2 + 2, :], v3(pab, 512), [pb], [xnTb[kp * 2], xnTb[kp * 2 + 1]])
            if s + 1 < NSLOT:
                load_xs(s + 1)
            ffn(lambda j: fetch(("M", s, j)), evac_y)
            store_tokmajor_f32(hT, lambda k: [hTb[k]], lambda tb: yg[s * 512 + tb * 128:s * 512 + (tb + 1) * 128, :], ygb, "sp")

        gfin = hT_all[:, 0:1024]
        dmas("sp", "gf", gfin, gfin_d[:, :], [], [hTb[0], hTb[1]])
        P4 = []
        regs = [(va_f, [VA]), (vb_f, [VB]),
                (arena[:, wsl_off:wsl_off + 4096], [wslb[0], wslb[1]]),
                (arena[:, wsl_off + 4096:wsl_off + 8192], [wslb[2], wslb[3]])]
        NSET = len(regs)
        for si, (reg, rb) in enumerate(regs):
            P4.append([(reg[:, i * 1024:(i + 1) * 1024], Buf(f"p4_{si}_{i}")) for i in range(4)])
        first = [True] * NSET
        exw = {}

        def p4_loads(bg):
            si = bg % NSET
            (h3a, h3bf), (y1a, y1b), (y2a, y2b), (oa, ob) = P4[si]
            ex_w = list(regs[si][1]) if first[si] else []
            first[si] = False
            exw[bg] = ex_w
            dmas("sp", f"a{si}0", h3a, h3_scr[bg * 128:(bg + 1) * 128, :], [h3b], [h3bf] + ex_w)
            for (ya, yb_, idx, key) in ((y1a, y1b, idx1i, f"a{si}1"), (y2a, y2b, idx2i, f"a{si}2")):
                fw.dma("pool", key, (lambda o_, i_, b_: lambda e: e.indirect_dma_start(
                    out=o_, out_offset=None, in_=yg[:, :],
                    in_offset=bass.IndirectOffsetOnAxis(ap=i_[:, b_:b_ + 1], axis=0)))(ya, idx, bg),
                    reads=[ygb, tabb], writes=[yb_] + ex_w)

        for bg in range(min(NSET - 1, NB)):
            p4_loads(bg)
        for bg in range(NB):
            if bg + NSET - 1 < NB:
                p4_loads(bg + NSET - 1)
            si = bg % NSET
            (h3a, h3bf), (y1a, y1b), (y2a, y2b), (oa, ob) = P4[si]
            ex_w = exw[bg]
            sq_ = ssq4[:, (bg % 4) * 4:(bg % 4) * 4 + 4]
            sqb_ = ssqhb[bg % 4]
            stt(h3a, y1a, g1[:, bg:bg + 1], h3a, ALU.mult, ALU.add, [y1b, tabb, h3bf], [h3bf])
            stt(h3a, y2a, g2[:, bg:bg + 1], h3a, ALU.mult, ALU.add, [y2b, tabb, h3bf], [h3bf])
            act(y1a, h3a, AF.Square, reads=[h3bf], writes=[y1b, sqb_], accum=sq_[:, 0:1])
            act(sq_[:, 1:2], sq_[:, 0:1], AF.Sqrt, reads=[sqb_, epsbb], writes=[sqb_], scale=1.0 / D, bias=epsb[:, 0:1])
            fw.op("dve", (lambda q_: lambda e: e.reciprocal(out=q_[:, 2:3], in_=q_[:, 1:2]))(sq_), reads=[sqb_], writes=[sqb_])
            stt(oa, h3a, sq_[:, 2:3], gfin, ALU.mult, ALU.mult, [h3bf, sqb_, hTb[0], hTb[1]], [ob] + ex_w)
            dmas("sp", f"a{si}3", out[bg * 128:(bg + 1) * 128, :], oa, [ob], [outb])

        fw.fence("sp", [P4[i][3][1] for i in range(NSET)] + wslb + iob + [outb])
        if dbg:
            fw.fence("sp", hTb)
        fw.emit_all(st)
        build_nc.stats = {e: len(fw.ops[e]) for e in ENGS}
        build_nc.stats["waits"] = fw.n_waits
    return nc


def host_consts(S):
    consts = np.zeros((128, 520), np.float64)
    j = np.arange(128)[:, None].astype(np.float64)
    i = np.arange(128)[None, :].astype(np.float64)
    for h in range(H):
        g = GAM[h]
        consts[:, h * 128:(h + 1) * 128] = np.where(i >= j, g ** (-(j + 1.0)), 0.0)
        consts[:, 512 + h] = g ** (np.arange(128) + 1.0)
        consts[:, 516 + h] = g ** (127.0 - np.arange(128))
    inv_freq = (np.float32(10000.0) ** (-(np.arange(0, 256, 2, dtype=np.float32)) / np.float32(256))).astype(np.float32)
    pos = np.arange(S, dtype=np.float32)
    ang = (inv_freq[:, None] * pos[None, :]).astype(np.float32).astype(np.float64)
    tabs = np.stack([np.cos(ang), np.sin(ang), np.cos(ang) / 16.0, np.sin(ang) / 16.0]).astype(np.float32)
    return consts.astype(np.float32), tabs


def host_small(inputs):
    small = np.zeros((128, 128), np.float32)
    for n, name in enumerate(["norm_mix0", "norm_ffn0", "norm_mix1", "norm_ffn1", "norm_final"]):
        small[:, n * 8:(n + 1) * 8] = np.asarray(inputs[name], np.float32).reshape(8, 128).T
    cw = np.asarray(inputs["conv_w"], np.float32)
    for jj in range(3):
        small[:, 40 + jj * 8:48 + jj * 8] = cw[jj].reshape(8, 128).T
    wr = np.asarray(inputs["moe_router"], np.float32)
    small[:, 64:128] = wr.reshape(8, 128, 8).transpose(1, 0, 2).reshape(128, 64)
    return small


WNAMES = ["ret_w_in", "ret_w_out", "ffn_w_gate", "ffn_w_up", "ffn_w_down", "conv_w_in", "conv_w_out",
          "moe_w_gate", "moe_w_up", "moe_w_down"]


def make_in_maps(inputs):
    x = np.asarray(inputs["x"], np.float32)
    B, S, _ = x.shape
    consts, tabs = host_consts(S)
    small = host_small(inputs)
    common = {n: np.ascontiguousarray(np.asarray(inputs[n], np.float32)) for n in WNAMES}
    common["small"] = small
    common["consts"] = consts
    common["tabs"] = tabs
    common["gfin"] = np.ascontiguousarray(np.broadcast_to(np.asarray(inputs["norm_final"], np.float32)[None, :], (128, D)))
    return [dict(common, x=np.ascontiguousarray(x[b])) for b in range(B)], B, S


def kernel(**inputs):
    in_maps, B, S = make_in_maps(inputs)
    nc = build_nc(S)
    res = run_bass_kernel_spmd(nc, in_maps, core_ids=list(range(B)))
    return np.stack([np.asarray(r["out"], np.float32) for r in res.results]).astype(np.float32)
```

```python
import contextlib
import numpy as np
import concourse.bass as bass
import concourse.mybir as mybir
from concourse.bass_utils import run_bass_kernel_spmd

F32 = mybir.dt.float32
BF16 = mybir.dt.bfloat16
AF = mybir.ActivationFunctionType
ALU = mybir.AluOpType
AX = mybir.AxisListType

ENGS = ("sp", "act", "dve", "pool", "pe")


class Buf:
    __slots__ = ("name", "ap", "writers", "readers", "disjoint")

    def __init__(self, name, ap=None, disjoint=False):
        self.name = name
        self.ap = ap
        self.disjoint = disjoint
        self.writers = {}
        self.readers = {}


class Op:
    __slots__ = ("emit", "deps", "marked", "dma")

    def __init__(self, emit, deps, dma):
        self.emit = emit
        self.deps = deps
        self.marked = False
        self.dma = dma


class FW:
    def __init__(self, nc):
        self.nc = nc
        self.ops = {e: [] for e in ENGS}
        self.dma_count = {}
        self.n_waits = 0

    def _collect(self, eng, reads, writes):
        deps = {}

        def add(tok):
            cls = (tok[0], tok[1])
            if deps.get(cls, -1) < tok[2]:
                deps[cls] = tok[2]

        for b in reads:
            for tok in b.writers.values():
                if tok[0] == "eng" and tok[1] == "pe" and eng == "pe":
                    continue
                add(tok)
        for b in writes:
            for d in (b.writers, b.readers):
                for tok in d.values():
                    if tok[0] == "eng" and tok[1] == eng:
                        continue
                    if d is b.writers and b.disjoint and tok[0] == "dma" and eng.startswith("dma:"):
                        continue
                    add(tok)
        return deps

    def _mark(self, deps):
        for (kind, k), v in deps.items():
            if kind == "eng":
                self.ops[k][v].marked = True

    def op(self, eng, emit, reads=(), writes=()):
        deps = self._collect(eng, reads, writes)
        self._mark(deps)
        idx = len(self.ops[eng])
        self.ops[eng].append(Op(emit, deps, None))
        tok = ("eng", eng, idx)
        for b in reads:
            b.readers[eng] = tok
        for b in writes:
            b.writers[eng] = tok

    def dma(self, eng, key, emit, reads=(), writes=()):
        cls = "dma:" + key
        deps = self._collect(cls, reads, writes)
        self._mark(deps)
        val = self.dma_count.get(key, 0) + 16
        self.dma_count[key] = val
        self.ops[eng].append(Op(emit, deps, (key, val)))
        tok = ("dma", key, val)
        for b in reads:
            b.readers[cls] = tok
        for b in writes:
            b.writers[cls] = tok

    def fence(self, eng, bufs):
        deps = self._collect(eng + "_fence", list(bufs), list(bufs))
        self._mark(deps)
        self.ops[eng].append(Op(None, deps, None))

    def emit_all(self, stack):
        nc = self.nc
        sems = {e: stack.enter_context(nc.semaphore("s_" + e)) for e in ENGS if e != "sp"}
        dsems = {k: stack.enter_context(nc.semaphore("d_" + k)) for k in self.dma_count}
        counts = {}
        for e in ENGS:
            c = 0
            arr = []
            for o in self.ops[e]:
                if o.marked:
                    c += 1
                arr.append(c)
            counts[e] = arr
        block = stack.enter_context(nc.Block())
        fw = self

        def run(e, engobj):
            water = {}
            for o in fw.ops[e]:
                for (kind, k), v in o.deps.items():
                    if kind == "eng":
                        sem = sems[k]
                        val = counts[k][v]
                    else:
                        sem = dsems[k]
                        val = v
                    wk = (kind, k)
                    if water.get(wk, 0) < val:
                        engobj.wait_ge(sem, val)
                        water[wk] = val
                        fw.n_waits += 1
                if o.emit is None:
                    continue
                inst = o.emit(engobj)
                if o.dma is not None:
                    inst.then_inc(dsems[o.dma[0]], 16)
                elif o.marked:
                    inst.then_inc(sems[e], 1)

        @block.sync
        def _(eng):
            run("sp", eng)

        @block.scalar
        def _(eng):
            run("act", eng)

        @block.vector
        def _(eng):
            run("dve", eng)

        @block.gpsimd
        def _(eng):
            run("pool", eng)

        @block.tensor
        def _(eng):
            run("pe", eng)


I32 = mybir.dt.int32
D = 1024
H = 4
FF = 3584
NE = 8
TT = 512
EPS = 1e-6
NW = 4
LA = NW - 2
GAM = [1.0 - 2.0 ** (-5.0 - h) for h in range(H)]


def build_nc(S, dbg=False):
    NT = S // TT
    NB = NT * 4
    NSLOT = (2 * S) // 512 + NE
    nc = bass.Bass("TRN2", target_bir_lowering=False)

    def din(name, shape, dt=F32):
        return nc.dram_tensor(name, list(shape), dt, kind="ExternalInput").ap()

    x = din("x", [S, D])
    ret_w_in = din("ret_w_in", [D, 6144])
    ret_w_out = din("ret_w_out", [2048, D])
    ffn_w_gate = din("ffn_w_gate", [D, FF])
    ffn_w_up = din("ffn_w_up", [D, FF])
    ffn_w_down = din("ffn_w_down", [FF, D])
    conv_w_in = din("conv_w_in", [D, 3072])
    conv_w_out = din("conv_w_out", [D, D])
    moe_w_gate = din("moe_w_gate", [NE, D, FF])
    moe_w_up = din("moe_w_up", [NE, D, FF])
    moe_w_down = din("moe_w_down", [NE, FF, D])
    small_d = din("small", [128, 128])
    consts_d = din("consts", [128, 520])
    tabs_d = din("tabs", [4, 128, S])
    gfin_d = din("gfin", [128, D])
    out = nc.dram_tensor("out", [S, D], F32, kind="ExternalOutput").ap()
    if dbg:
        dbg_d = nc.dram_tensor("dbg", [4, 8, 128, S], F32, kind="ExternalOutput").ap()

    blocks = []

    def addA(w, c0):
        blocks.append(("A", w, c0))
        return len(blocks) - 1

    def addB(w, c0):
        blocks.append(("B", w, c0))
        return len(blocks) - 1

    def addC(w, c0):
        blocks.append(("C", w, c0))
        return len(blocks) - 1

    bl_qk = [addA(ret_w_in, c * 512) for c in range(4)]
    bl_v = [addA(ret_w_in, 2048 + c * 512) for c in range(4)]
    bl_g = [addA(ret_w_in, 4096 + c * 512) for c in range(4)]
    bl_wo = [addB(ret_w_out, c * 256) for c in range(4)]
    bl_ffn = []
    for fb in range(7):
        bl_ffn.append(addA(ffn_w_gate, fb * 512))
        bl_ffn.append(addA(ffn_w_up, fb * 512))
    for dc in range(8):
        bl_ffn.append(addC(ffn_w_down, dc * 128))
    bl_conv = []
    for half in range(2):
        bl_conv.append((addA(conv_w_in, 1024 + half * 512), addA(conv_w_in, 2048 + half * 512),
                        addA(conv_w_in, half * 512)))
    bl_co = [addA(conv_w_out, c * 512) for c in range(2)]
    NBLK1 = len(blocks)
    moe_blocks = []
    for e in range(NE):
        for fb in range(7):
            moe_blocks.append(("A", moe_w_gate[e], fb * 512))
            moe_blocks.append(("A", moe_w_up[e], fb * 512))
        for dc in range(8):
            moe_blocks.append(("C", moe_w_down[e], dc * 128))
    NROWS = (NBLK1 + len(moe_blocks)) * 128
    wscr = nc.dram_tensor("wscr", [NROWS, 4096], BF16, kind="Internal").ap()
    h3_scr = nc.dram_tensor("h3_scr", [S, D], F32, kind="Internal").ap()
    xn_scr = nc.dram_tensor("xn_scr", [S, D], BF16, kind="Internal").ap()
    xg = nc.dram_tensor("xg", [NSLOT * 512, D], BF16, kind="Internal").ap()
    yg = nc.dram_tensor("yg", [NSLOT * 512, D], F32, kind="Internal").ap()
    scrb = [Buf(f"scr{i}") for i in range(NBLK1)]
    moescrb = Buf("moescr")
    h3b = Buf("h3scr")
    xnsb = Buf("xnscr")
    xgz = Buf("xgz", disjoint=True)
    xgs = Buf("xgs", disjoint=True)
    ygb = Buf("yg")
    outb = Buf("outdram", disjoint=True)

    st = contextlib.ExitStack()
    with st:
        fw = FW(nc)
        ARENA = 47800
        arena = st.enter_context(nc.sbuf_tensor("arena", [128, ARENA], F32))
        off = [0]

        def carve(n):
            a = arena[:, off[0]:off[0] + n]
            off[0] += n
            assert off[0] <= ARENA, off[0]
            return a

        def v3(ap, b):
            return ap.rearrange("p (a b) -> p a b", b=b)

        hT_all = carve(4096)
        hT = v3(hT_all, 512)
        hTb = [Buf(f"hT{k}") for k in range(8)]
        xnT_all = carve(2048).bitcast(BF16)
        xnT = v3(xnT_all, 512)
        xnTb = [Buf(f"xnT{k}") for k in range(8)]
        io_ap = [carve(1024), carve(1024)]
        iob = [Buf("io0"), Buf("io1")]
        tab_all = carve(2048)
        tab = v3(tab_all, 512)
        tabb = Buf("tab")
        qk_f = carve(4096)
        xin = v3(qk_f, 1024)
        qk_all = qk_f.bitcast(BF16)
        qT = v3(qk_all[:, 0:4096], 512)
        kT = v3(qk_all[:, 4096:8192], 512)
        ogT = v3(qk_all, 512)
        yT = qT
        QA = Buf("QA")
        QB = Buf("QB")
        ks_all = carve(2048).bitcast(BF16)
        ktok = v3(ks_all, 1024)
        sq_all = ks_all
        sq = v3(ks_all, 512)
        xs3 = ktok
        KS = Buf("KS")
        va_f = carve(4224)
        VA = Buf("VA")
        vtok = v3(va_f[:, 0:4096].bitcast(BF16), 2048)
        uT = v3(va_f[:, 0:4112], 514)
        xnf = uT[:, :, 2:514]
        hffA = v3(va_f[:, 0:4096].bitcast(BF16), 512)
        vb_f = carve(4096)
        VB = Buf("VB")
        gtok = v3(vb_f.bitcast(BF16), 2048)
        hffB = v3(vb_f.bitcast(BF16), 512)
        stF = [v3(carve(1024), 512) for _ in range(H)]
        stFb = [[Buf(f"stF{h}_{dc}") for dc in range(2)] for h in range(H)]
        stB = [v3(carve(512).bitcast(BF16), 512) for _ in range(H)]
        stBb = [[Buf(f"stB{h}_{dc}") for dc in range(2)] for h in range(H)]
        halo = v3(carve(16), 2)
        halob = Buf("halo")
        wsl_off = off[0]
        wsl = [carve(2048).bitcast(BF16) for _ in range(NW)]
        wslb = [Buf(f"ws{i}") for i in range(NW)]
        NTMP = 6
        tmp_all = carve(512 * NTMP)
        tmp_ap = [tmp_all[:, i * 512:(i + 1) * 512] for i in range(NTMP)]
        tmpb = [Buf(f"tmp{i}") for i in range(NTMP)]
        rstd = carve(512)
        rstdb = Buf("rstd")
        consts = carve(520)
        constb = Buf("consts")
        small = carve(128)
        smallb = Buf("small")
        identf = carve(128)
        identfb = Buf("identf")
        iot = carve(128)
        identb = carve(64).bitcast(BF16)
        identbb = Buf("identb")
        onesb = carve(64).bitcast(BF16)
        onesbb = Buf("onesb")
        lstrb = carve(64).bitcast(BF16)
        lstrbb = Buf("lstr")
        epsb = carve(1)
        epsbb = Buf("eps")
        sT_ap = [carve(256).bitcast(BF16), carve(256).bitcast(BF16)]
        sTb = [Buf("sT0"), Buf("sT1")]
        ssq4 = carve(16)
        ssqhb = [Buf(f"ssqh{h}") for h in range(H)]
        junk = carve(256).bitcast(BF16)
        junkb = Buf("junk")
        ssq = carve(4)
        ssqb = Buf("ssq")
        lg = carve(32)
        lgb = Buf("lg")
        m8 = carve(32)
        m8b = Buf("m8")
        rt = carve(64)
        rtb = Buf("rt")
        gA = carve(NB * 8)
        gAb = Buf("gA")
        s1A = carve(NB * 8)
        s1b = Buf("s1A")
        p2s = carve(256)
        P2 = Buf("P2")
        zt = carve(512).bitcast(BF16)
        ztb = Buf("zt")

        PS = []
        for i in range(8):
            PS.append((st.enter_context(nc.psum_tensor(f"ps{i}", [128, 512], F32))[:, :], Buf(f"ps{i}")))
        psi = [0]

        def nps():
            p = PS[psi[0] % 8]
            psi[0] += 1
            return p

        tmi = [0]

        def ntmp():
            i = tmi[0] % NTMP
            tmi[0] += 1
            return tmp_ap[i], tmpb[i], i

        Mt = consts[:, 0:512]
        qdec = consts[:, 512:516]
        kdec = consts[:, 516:520]

        def mm(psap, psbuf, lhsT, rhs, start, stop, reads):
            fw.op("pe", lambda e: e.matmul(psap, lhsT=lhsT, rhs=rhs, start=start, stop=stop),
                  reads=reads, writes=[psbuf])

        def tr(psap, psbuf, in_, ident, reads):
            fw.op("pe", lambda e: e.transpose(psap, in_=in_, identity=ident), reads=reads, writes=[psbuf])

        def act(out_, in_, func, reads, writes, scale=None, bias=None, accum=None):
            kw = {}
            if scale is not None:
                kw["scale"] = scale
            if bias is not None:
                kw["bias"] = bias
            if accum is not None:
                kw["accum_out"] = accum
            fw.op("act", lambda e: e.activation(out=out_, in_=in_, func=func, **kw), reads=reads, writes=writes)

        def tt(eng, out_, in0, in1, op, reads, writes):
            fw.op(eng, lambda e: e.tensor_tensor(out=out_, in0=in0, in1=in1, op=op), reads=reads, writes=writes)

        def stt(out_, in0, scalar, in1, op0, op1, reads, writes):
            fw.op("dve", lambda e: e.scalar_tensor_tensor(out=out_, in0=in0, scalar=scalar, in1=in1, op0=op0, op1=op1),
                  reads=reads, writes=writes)

        def ts(eng, out_, in0, s1, op0, reads, writes, s2=None, op1=None):
            if op1 is None:
                fw.op(eng, lambda e: e.tensor_scalar(out=out_, in0=in0, scalar1=s1, scalar2=None, op0=op0),
                      reads=reads, writes=writes)
            else:
                fw.op(eng, lambda e: e.tensor_scalar(out=out_, in0=in0, scalar1=s1, scalar2=s2, op0=op0, op1=op1),
                      reads=reads, writes=writes)

        def cp(eng, out_, in_, reads, writes):
            fw.op(eng, lambda e: e.tensor_copy(out=out_, in_=in_), reads=reads, writes=writes)

        def red(out_, in_, reads, writes):
            fw.op("dve", lambda e: e.tensor_reduce(out=out_, in_=in_, axis=AX.X, op=ALU.add), reads=reads, writes=writes)

        def dmas(eng, key, out_, in_, reads, writes):
            fw.dma(eng, key, lambda e: e.dma_start(out=out_, in_=in_), reads=reads, writes=writes)

        dmas("sp", "cst", consts, consts_d[:, :], [], [constb])
        dmas("sp", "cst2", small, small_d[:, :], [], [smallb])
        iotb = Buf("iot")
        fw.op("pool", lambda e: e.iota(iot, pattern=[[1, 128]], base=0, channel_multiplier=-1,
                                       allow_small_or_imprecise_dtypes=True), writes=[iotb])
        fw.op("dve", lambda e: e.tensor_single_scalar(out=identf, in_=iot, scalar=0.0, op=ALU.is_equal),
              reads=[iotb], writes=[identfb])
        fw.op("dve", lambda e: e.tensor_single_scalar(out=identb, in_=iot, scalar=0.0, op=ALU.is_equal),
              reads=[iotb], writes=[identbb])
        fw.op("dve", lambda e: e.tensor_single_scalar(out=lstrb, in_=iot, scalar=0.0, op=ALU.is_gt),
              reads=[iotb], writes=[lstrbb])
        fw.op("dve", lambda e: e.memset(onesb, 1.0), writes=[onesbb])
        fw.op("dve", lambda e: e.memset(epsb, EPS), writes=[epsbb])
        for h in range(H):
            fw.op("pool", (lambda hh: lambda e: e.memset(stF[hh], 0.0))(h), writes=stFb[h])
            fw.op("pool", (lambda hh: lambda e: e.memset(stB[hh], 0.0))(h), writes=stBb[h])
        fw.op("pool", lambda e: e.memset(halo, 0.0), writes=[halob])
        fw.op("pool", lambda e: e.memset(zt, 0.0), writes=[ztb])

        seq = [("R", t, bi) for t in range(NT) for bi in range(NBLK1)]
        N1 = len(seq)
        seq += [("M", s, j) for s in range(NSLOT) for j in range(22)]
        issued = [0]
        barrier = [N1]
        widx_ref = [None, None]

        def blk_n(kind):
            return 4096 if kind in ("A", "B") else 3584

        def src_view(kind, w, c0):
            if kind == "A":
                return w[:, c0:c0 + 512].rearrange("(k p) j -> p k j", p=128), 512
            if kind == "B":
                return w[:, c0:c0 + 256].rearrange("(k p) j -> p k j", p=128), 256
            return w[:, c0:c0 + 128].rearrange("(k p) j -> p k j", p=128), 128

        def issue_load(g):
            ent = seq[g]
            si = g % NW
            slot = wsl[si]
            sb = wslb[si]
            if ent[0] == "M":
                _, s, j = ent
                col = s * 22 + j
                widx, widxb = widx_ref
                fw.dma("pool", f"w{si}", lambda e: e.indirect_dma_start(
                    out=slot, out_offset=None, in_=wscr[:, :],
                    in_offset=bass.IndirectOffsetOnAxis(ap=widx[:, col:col + 1], axis=0)), reads=[moescrb, widxb], writes=[sb])
                return
            _, t, bi = ent
            kind, w, c0 = blocks[bi]
            n = blk_n(kind)
            rows = wscr[bi * 128:(bi + 1) * 128, 0:n]
            if t == 0:
                src, jw = src_view(kind, w, c0)
                dst = v3(slot[:, 0:n], jw)
                if kind == "C":
                    dmas("pool", f"w{si}", dst[:, 0:14, :], src[:, 0:14, :], [], [sb])
                    dmas("pool", f"w{si}", dst[:, 14:28, :], src[:, 14:28, :], [], [sb])
                else:
                    dmas("pool", f"w{si}", dst, src, [], [sb])
                if NT > 1:
                    dmas("sp", f"s{si}", rows, slot[:, 0:n], [sb], [scrb[bi]])
            else:
                dmas("sp", f"w{si}", slot[:, 0:n], rows, [scrb[bi]], [sb])

        gctr = [0]

        def fetch(key):
            g = gctr[0]
            assert seq[g] == key, (seq[g], key)
            gctr[0] += 1
            while issued[0] <= min(g + LA, barrier[0] - 1):
                issue_load(issued[0])
                issued[0] += 1
            si = g % NW
            return wsl[si], wslb[si]

        cv_next = [0]
        cv_per_fetch = -(-len(moe_blocks) // max(1, (NT - 1) * NBLK1))
        cv_stride = max(1, ((NT - 1) * NBLK1) // len(moe_blocks))
        cv_tick = [0]

        def cv_some(n):
            for _ in range(n):
                i = cv_next[0]
                if i >= len(moe_blocks):
                    return
                cv_next[0] += 1
                kind, w, c0 = moe_blocks[i]
                src, jw = src_view(kind, w, c0)
                nn = blk_n(kind)
                r0 = (NBLK1 + i) * 128
                dst = wscr[r0:r0 + 128, 0:nn].rearrange("p (k j) -> p k j", j=jw)
                if kind == "C":
                    dmas("pool", "cv", dst[:, 0:14, :], src[:, 0:14, :], [], [moescrb])
                    dmas("pool", "cv", dst[:, 14:28, :], src[:, 14:28, :], [], [moescrb])
                else:
                    dmas("pool", "cv", dst, src, [], [moescrb])

        zf_next = [0]

        def zf_some(n):
            for _ in range(n):
                r = zf_next[0]
                if r >= NSLOT * 4:
                    return
                zf_next[0] += 1
                dmas("sp", "zf", xg[r * 128:(r + 1) * 128, :], zt, [ztb], [xgz])

        def fetch1(t, bi):
            if t >= 1 or NT == 1:
                cv_tick[0] += 1
                if cv_tick[0] % cv_stride == 0:
                    cv_some(cv_per_fetch)
                zf_some(1 if NT > 4 else 8)
            return fetch(("R", t, bi))

        def hff(f):
            return (hffA[:, f, :], VA) if f < 16 else (hffB[:, f - 16, :], VB)

        def sq_chunk(k):
            act(sq[:, k, :], hT[:, k, :], AF.Square, reads=[hTb[k]], writes=[KS])

        def rmsnorm(nidx, f32out=False, presq=True):
            if not presq:
                act(sq_all, hT_all, AF.Square, reads=hTb, writes=[KS])
            pa, pb = nps()
            for k in range(8):
                mm(pa, pb, onesb, sq[:, k, :], k == 0, k == 7, [onesbb, KS])
            act(rstd, pa, AF.Ln, reads=[pb, epsbb], writes=[rstdb], scale=1.0 / D, bias=epsb[:, 0:1])
            act(rstd, rstd, AF.Exp, reads=[rstdb], writes=[rstdb], scale=-0.5)
            for k in range(8):
                g = small[:, nidx * 8 + k:nidx * 8 + k + 1]
                if f32out:
                    stt(xnf[:, k, :], hT[:, k, :], g, rstd, ALU.mult, ALU.mult, [hTb[k], smallb, rstdb], [VA])
                else:
                    stt(xnT[:, k, :], hT[:, k, :], g, rstd, ALU.mult, ALU.mult, [hTb[k], smallb, rstdb], [xnTb[k]])

        def dump(stage, t):
            if dbg:
                dst = dbg_d[stage, :, :, t * TT:(t + 1) * TT].rearrange("k p s -> p k s")
                dmas("pool", "dbg", dst, hT, hTb, [])

        def resid_add(dchunk, pa, pb):
            tt("dve", hT[:, dchunk, :], pa, hT[:, dchunk, :], ALU.add, [pb, hTb[dchunk]], [hTb[dchunk]])
            sq_chunk(dchunk)

        def ffn(fetchj, evac):
            for fb in range(7):
                sg, sgb = fetchj(2 * fb)
                su, sub = fetchj(2 * fb + 1)
                sg3 = v3(sg, 512)
                su3 = v3(su, 512)
                for fc in range(4):
                    f = fb * 4 + fc
                    pga, pgb = nps()
                    pua, pub = nps()
                    for k in range(8):
                        mm(pga, pgb, sg3[:, k, fc * 128:(fc + 1) * 128], xnT[:, k, :], k == 0, k == 7, [sgb, xnTb[k]])
                    for k in range(8):
                        mm(pua, pub, su3[:, k, fc * 128:(fc + 1) * 128], xnT[:, k, :], k == 0, k == 7, [sub, xnTb[k]])
                    ta, tb_, _ = ntmp()
                    act(ta, pga, AF.Silu, reads=[pgb], writes=[tb_])
                    ha, hb = hff(f)
                    tt("dve", ha, pua, ta, ALU.mult, [pub, tb_], [hb])
            for dc in range(8):
                sd, sdb = fetchj(14 + dc)
                sd3 = v3(sd[:, 0:3584], 128)
                pa, pb = nps()
                for f in range(28):
                    ha, hb = hff(f)
                    mm(pa, pb, sd3[:, f, :], ha, f == 0, f == 27, [sdb, hb])
                evac(dc, pa, pb)

        def store_tokmajor_f32(srcT, src_reads, dram_rows, dram_buf, queue):
            for tb in range(4):
                io = io_ap[tb % 2]
                ib = iob[tb % 2]
                for half in range(2):
                    pa, pb = nps()
                    for kk in range(4):
                        k = half * 4 + kk
                        tr(pa[:, kk * 128:(kk + 1) * 128], pb, srcT[:, k, tb * 128:(tb + 1) * 128], identf,
                           src_reads(k) + [identfb])
                    if half == 0:
                        act(io[:, 0:512], pa, AF.Copy, reads=[pb], writes=[ib])
                    else:
                        cp("dve", io[:, 512:1024], pa, [pb], [ib])
                dmas(queue, f"io{tb % 2}", dram_rows(tb), io, [ib], [dram_buf])

        def load_x(t_, half):
            r0 = t_ * TT + half * 256
            src = x[r0:r0 + 256, :].rearrange("(tb p) d -> p tb d", p=128)
            dmas("sp", f"xin{half}", xin[:, half * 2:half * 2 + 2, :], src, [], [QA if half == 0 else QB])

        for t in range(NT):
            tok0 = t * TT
            tsrc = tabs_d[:, :, tok0:tok0 + TT].rearrange("a p s -> p a s")
            dmas("sp", "tab", tab, tsrc, [], [tabb])
            if t == 0:
                load_x(0, 1)
                load_x(0, 0)
            for tb in (2, 3, 0, 1):
                xb_ = QA if tb < 2 else QB
                for half in range(2):
                    pa, pb = nps()
                    for kk in range(4):
                        k = half * 4 + kk
                        tr(pa[:, kk * 128:(kk + 1) * 128], pb, xin[:, tb, k * 128:(k + 1) * 128], identf, [xb_, identfb])
                    dst = hT[:, half * 4:(half + 1) * 4, tb * 128:(tb + 1) * 128]
                    srcv = v3(pa, 128)
                    wr = [hTb[half * 4 + kk] for kk in range(4)]
                    if half == 0:
                        act(dst, srcv, AF.Copy, reads=[pb], writes=wr)
                    else:
                        cp("dve", dst, srcv, [pb], wr)
                act(sq[:, :, tb * 128:(tb + 1) * 128], hT[:, :, tb * 128:(tb + 1) * 128], AF.Square, reads=hTb, writes=[KS])
            dump(0, t)
            rmsnorm(0)
            for blk in range(4):
                sl, slb = fetch1(t, bl_qk[blk])
                sl3 = v3(sl, 512)
                isq = blk < 2
                for hh in range(2):
                    h = (blk % 2) * 2 + hh
                    p1a, p1b = nps()
                    p2a, p2b = nps()
                    for k in range(8):
                        mm(p1a, p1b, sl3[:, k, (hh * 2) * 128:(hh * 2 + 1) * 128], xnT[:, k, :], k == 0, k == 7, [slb, xnTb[k]])
                    for k in range(8):
                        mm(p2a, p2b, sl3[:, k, (hh * 2 + 1) * 128:(hh * 2 + 2) * 128], xnT[:, k, :], k == 0, k == 7, [slb, xnTb[k]])
                    cos = tab[:, 0 if isq else 2, :]
                    sin = tab[:, 1 if isq else 3, :]
                    dstT = qT if isq else kT
                    dbuf = QA if isq else QB
                    aa, ab, _ = ntmp()
                    ba, bb, _ = ntmp()
                    ca, cb, _ = ntmp()
                    da, db, _ = ntmp()
                    tt("dve", aa, p1a, cos, ALU.mult, [p1b, tabb], [ab])
                    tt("dve", ba, p2a, sin, ALU.mult, [p2b, tabb], [bb])
                    tt("dve", ca, p1a, sin, ALU.mult, [p1b, tabb], [cb])
                    tt("dve", da, p2a, cos, ALU.mult, [p2b, tabb], [db])
                    tt("dve", dstT[:, h * 2, :], aa, ba, ALU.subtract, [ab, bb], [dbuf])
                    tt("dve", dstT[:, h * 2 + 1, :], ca, da, ALU.add, [cb, db], [dbuf])
            def ktok_transposes():
                for tb in range(4):
                    pa, pb = nps()
                    pab = pa.bitcast(BF16)
                    for c8 in range(8):
                        tr(pab[:, c8 * 128:(c8 + 1) * 128], pb, kT[:, c8, tb * 128:(tb + 1) * 128], identb, [QB, identbb])
                    for h in range(H):
                        act(ktok[:, tb, h * 256:(h + 1) * 256], pab[:, h * 256:(h + 1) * 256], AF.Copy,
                            reads=[pb, constb], writes=[KS], scale=kdec[:, h:h + 1])

            for h in range(H):
                sl, slb = fetch1(t, bl_v[h])
                sl3 = v3(sl, 512)
                for tb in range(4):
                    pa, pb = nps()
                    for k in range(8):
                        mm(pa, pb, xnT[:, k, tb * 128:(tb + 1) * 128], sl3[:, k, :], k == 0, k == 7, [slb, xnTb[k]])
                    cp("dve", vtok[:, tb, h * 512:(h + 1) * 512], pa, [pb], [VA])
                if h == 0:
                    ktok_transposes()
            for h in range(H):
                sl, slb = fetch1(t, bl_g[h])
                sl3 = v3(sl, 512)
                for tb in range(4):
                    pa, pb = nps()
                    for k in range(8):
                        mm(pa, pb, xnT[:, k, tb * 128:(tb + 1) * 128], sl3[:, k, :], k == 0, k == 7, [slb, xnTb[k]])
                    act(gtok[:, tb, h * 512:(h + 1) * 512], pa, AF.Silu, reads=[pb], writes=[VB])
            def stageA(c):
                cs = slice(c * 128, (c + 1) * 128)
                psa, psb_ = nps()
                for h in range(H):
                    for dc in range(2):
                        mm(psa[:, h * 128:(h + 1) * 128], psb_, kT[:, h * 2 + dc, cs], qT[:, h * 2 + dc, cs], dc == 0, dc == 1, [QA, QB])
                tt("dve", sT_ap[c % 2], psa, Mt, ALU.mult, [psb_, constb], [sTb[c % 2]])

            def stageB(c):
                cs = slice(c * 128, (c + 1) * 128)
                for h in range(H):
                    if h == 2 and c + 1 < 4:
                        stageA(c + 1)
                    hs = slice(h * 512, (h + 1) * 512)
                    sq_ = ssq4[:, h * 4:(h + 1) * 4]
                    poa, pob = nps()
                    mm(poa, pob, sT_ap[c % 2][:, h * 128:(h + 1) * 128], vtok[:, c, hs], True, False, [sTb[c % 2], VA])
                    for dc in range(2):
                        mm(poa, pob, qT[:, h * 2 + dc, cs], stB[h][:, dc, :], False, dc == 1, [QA, stBb[h][dc]])
                    pts = []
                    for dc in range(2):
                        pta, ptb = nps()
                        mm(pta, ptb, ktok[:, c, h * 256 + dc * 128:h * 256 + (dc + 1) * 128], vtok[:, c, hs], True, True, [KS, VA])
                        pts.append((pta, ptb))
                    act(junk, poa, AF.Square, reads=[pob, constb], writes=[junkb, ssqhb[h]], scale=qdec[:, h:h + 1], accum=sq_[:, 0:1])
                    act(sq_[:, 1:2], sq_[:, 0:1], AF.Sqrt, reads=[ssqhb[h], epsbb], writes=[ssqhb[h]], scale=1.0 / 512, bias=epsb[:, 0:1])
                    for dc in range(2):
                        pta, ptb = pts[dc]
                        stt(stF[h][:, dc, :], stF[h][:, dc, :], float(GAM[h] ** 128), pta, ALU.mult, ALU.add,
                            [stFb[h][dc], ptb], [stFb[h][dc]])
                        act(stB[h][:, dc, :], stF[h][:, dc, :], AF.Copy, reads=[stFb[h][dc]], writes=[stBb[h][dc]])
                    fw.op("dve", (lambda q_: lambda e: e.reciprocal(out=q_[:, 2:3], in_=q_[:, 1:2]))(sq_), reads=[ssqhb[h]], writes=[ssqhb[h]])
                    tt("dve", sq_[:, 3:4], sq_[:, 2:3], qdec[:, h:h + 1], ALU.mult, [ssqhb[h], constb], [ssqhb[h]])
                    stt(gtok[:, c, hs], poa, sq_[:, 3:4], gtok[:, c, hs], ALU.mult, ALU.mult, [pob, ssqhb[h], VB], [VB])

            stageA(0)
            for c in range(4):
                stageB(c)
            for ec2 in range(8):
                pa, pb = nps()
                pab = pa.bitcast(BF16)
                for e2 in range(2):
                    ec = ec2 * 2 + e2
                    for tb in range(4):
                        tr(pab[:, e2 * 512 + tb * 128:e2 * 512 + (tb + 1) * 128], pb, gtok[:, tb, ec * 128:(ec + 1) * 128],
                           identb, [VB, identbb])
                dst = ogT[:, ec2 * 2:ec2 * 2 + 2, :]
                srcv = v3(pab, 512)
                qb = QA if ec2 < 4 else QB
                if ec2 % 2 == 0:
                    act(dst, srcv, AF.Copy, reads=[pb], writes=[qb])
                else:
                    cp("dve", dst, srcv, [pb], [qb])
            for blk in range(4):
                sl, slb = fetch1(t, bl_wo[blk])
                sl3 = v3(sl, 256)
                for dd in range(2):
                    dch = blk * 2 + dd
                    pa, pb = nps()
                    for ek in range(16):
                        mm(pa, pb, sl3[:, ek, dd * 128:(dd + 1) * 128], ogT[:, ek, :], ek == 0, ek == 15,
                           [slb, QA if ek < 8 else QB])
                    resid_add(dch, pa, pb)
            if t + 1 < NT:
                load_x(t + 1, 1)
            dump(1, t)
            rmsnorm(1)
            ffn(lambda j: fetch1(t, bl_ffn[j]), resid_add)
            dump(2, t)
            rmsnorm(2)
            cp("dve", uT[:, :, 0:2], halo, [halob], [VA])
            for half in range(2):
                bc, bh, bb_ = bl_conv[half]
                sc, scb = fetch1(t, bc)
                sh, shb = fetch1(t, bh)
                sc3 = v3(sc, 512)
                sh3 = v3(sh, 512)
                for d4 in range(4):
                    dch = half * 4 + d4
                    pca, pcb = nps()
                    pha, phb = nps()
                    for k in range(8):
                        mm(pca, pcb, sc3[:, k, d4 * 128:(d4 + 1) * 128], xnT[:, k, :], k == 0, k == 7, [scb, xnTb[k]])
                    for k in range(8):
                        mm(pha, phb, sh3[:, k, d4 * 128:(d4 + 1) * 128], xnT[:, k, :], k == 0, k == 7, [shb, xnTb[k]])
                    ta, tb_, _ = ntmp()
                    act(ta, pca, AF.Copy, reads=[pcb], writes=[tb_])
                    tt("dve", uT[:, dch, 2:514], pha, ta, ALU.mult, [phb, tb_], [VA])
                    ya = io_ap[d4 // 2][:, (d4 % 2) * 512:(d4 % 2 + 1) * 512]
                    yb = iob[d4 // 2]
                    ts("dve", ya, uT[:, dch, 0:512], small[:, 40 + dch:41 + dch], ALU.mult, [VA, smallb], [yb])
                    stt(ya, uT[:, dch, 1:513], small[:, 48 + dch:49 + dch], ya, ALU.mult, ALU.add, [VA, smallb, yb], [yb])
                    stt(ya, uT[:, dch, 2:514], small[:, 56 + dch:57 + dch], ya, ALU.mult, ALU.add, [VA, smallb, yb], [yb])
                sb_, sbb = fetch1(t, bb_)
                sb3 = v3(sb_, 512)
                for d4 in range(4):
                    dch = half * 4 + d4
                    pba, pbb = nps()
                    for k in range(8):
                        mm(pba, pbb, sb3[:, k, d4 * 128:(d4 + 1) * 128], xnT[:, k, :], k == 0, k == 7, [sbb, xnTb[k]])
                    ya = io_ap[d4 // 2][:, (d4 % 2) * 512:(d4 % 2 + 1) * 512]
                    yb = iob[d4 // 2]
                    tt("dve", yT[:, dch, :], pba, ya, ALU.mult, [pbb, yb], [QA])
            cp("dve", halo, uT[:, :, 512:514], [VA], [halob])
            for blk in range(2):
                sl, slb = fetch1(t, bl_co[blk])
                sl3 = v3(sl, 512)
                for d4 in range(4):
                    dch = blk * 4 + d4
                    pa, pb = nps()
                    for k in range(8):
                        mm(pa, pb, sl3[:, k, d4 * 128:(d4 + 1) * 128], yT[:, k, :], k == 0, k == 7, [slb, QA])
                    resid_add(dch, pa, pb)
            dump(3, t)
            if t + 1 < NT:
                load_x(t + 1, 0)
            rmsnorm(3, f32out=True)
            store_tokmajor_f32(hT, lambda k: [hTb[k]], lambda tb: h3_scr[tok0 + tb * 128:tok0 + (tb + 1) * 128, :], h3b, "sp")
            for k in range(0, 8, 2):
                act(xnT[:, k:k + 2, :], xnf[:, k:k + 2, :], AF.Copy, reads=[VA], writes=[xnTb[k], xnTb[k + 1]])
            pa, pb = nps()
            for tb in range(4):
                for k in range(8):
                    mm(pa[:, tb * 8:(tb + 1) * 8], pb, xnf[:, k, tb * 128:(tb + 1) * 128], small[:, 64 + k * 8:72 + k * 8],
                       k == 0, k == 7, [VA, smallb])
            cp("dve", lg, pa[:, 0:32], [pb], [lgb])
            for tb in range(4):
                l = lg[:, tb * 8:(tb + 1) * 8]
                m = m8[:, tb * 8:(tb + 1) * 8]
                mask = rt[:, 0:8]
                negm = rt[:, 8:9]
                ex = rt[:, 16:24]
                em = rt[:, 24:32]
                den = rt[:, 32:33]
                gcol = (t * 4 + tb) * 8
                fw.op("dve", (lambda m_, l_: lambda e: e.max(out=m_, in_=l_))(m, l), reads=[lgb], writes=[m8b])
                ts("dve", mask, l, m[:, 1:2], ALU.is_ge, [lgb, m8b], [rtb])
                ts("dve", s1A[:, gcol:gcol + 8], l, m[:, 0:1], ALU.is_equal, [lgb, m8b], [s1b])
                ts("dve", negm, m[:, 0:1], -1.0, ALU.mult, [m8b], [rtb])
                act(ex, l, AF.Exp, reads=[lgb, rtb], writes=[rtb], bias=negm, scale=1.0)
                tt("dve", em, ex, mask, ALU.mult, [rtb], [rtb])
                red(den, em, [rtb], [rtb])
                fw.op("dve", (lambda d_: lambda e: e.reciprocal(out=d_, in_=d_))(den), reads=[rtb], writes=[rtb])
                ts("dve", gA[:, gcol:gcol + 8], em, den, ALU.mult, [rtb], [gAb])
            for tb in range(4):
                pa, pb = nps()
                pab = pa.bitcast(BF16)
                for k in range(8):
                    tr(pab[:, k * 128:(k + 1) * 128], pb, xnT[:, k, tb * 128:(tb + 1) * 128], identb, [xnTb[k], identbb])
                ta, tb_, ti = ntmp()
                tab16 = ta.bitcast(BF16)
                if tb % 2 == 0:
                    act(tab16, pab, AF.Copy, reads=[pb], writes=[tb_])
                else:
                    cp("dve", tab16, pab, [pb], [tb_])
                dmas("sp", f"tm{ti}", xn_scr[tok0 + tb * 128:tok0 + (tb + 1) * 128, :], tab16, [tb_], [xnsb])
        cv_some(len(moe_blocks))
        zf_some(NSLOT * 4)

        W8 = NB * 8
        P2W = [P2] + tmpb + [rstdb, junkb]
        Mf = tmp_ap[0][:, 0:W8]
        csA = tmp_ap[1][:, 0:W8]
        csB = tmp_ap[2][:, 0:W8]
        pos = tmp_ap[3][:, 0:W8]
        dstf = tmp_ap[4][:, 0:W8]
        prod = tmp_ap[5][:, 0:W8]
        m2f = rstd[:, 0:W8]
        Mb16 = junk[:, 0:W8]

        def p2op(fn, extra_r=(), extra_w=()):
            fw.op("dve", fn, reads=[P2, gAb, s1b] + list(extra_r), writes=P2W + list(extra_w))

        p2op(lambda e: e.tensor_single_scalar(out=Mf, in_=gA, scalar=0.0, op=ALU.is_gt))
        p2op(lambda e: e.tensor_copy(out=Mb16, in_=Mf))
        p1a, p1b = nps()
        p2a, p2b = nps()
        mm(p1a[:, 0:W8], p1b, lstrb, Mb16, True, True, [lstrbb, P2])
        mm(p2a[:, 0:W8], p2b, onesb, Mb16, True, True, [onesbb, P2])
        p2op(lambda e: e.tensor_copy(out=csA, in_=p2a[:, 0:W8]), extra_r=[p2b])
        src_, dst_ = csA, csB
        k = 1
        while k < NB:
            s3, d3 = v3(src_, 8), v3(dst_, 8)
            p2op((lambda d3_, s3_, k_: lambda e: e.tensor_tensor(out=d3_[:, k_:, :], in0=s3_[:, k_:, :], in1=s3_[:, :NB - k_, :], op=ALU.add))(d3, s3, k))
            p2op((lambda d3_, s3_, k_: lambda e: e.tensor_copy(out=d3_[:, :k_, :], in_=s3_[:, :k_, :]))(d3, s3, k))
            src_, dst_ = dst_, src_
            k *= 2
        incl = src_
        incl3 = v3(incl, 8)
        pos3 = v3(pos, 8)
        lp3 = v3(p1a[:, 0:W8], 8)
        p2op(lambda e: e.tensor_copy(out=pos3[:, 0:1, :], in_=lp3[:, 0:1, :]), extra_r=[p1b])
        if NB > 1:
            p2op(lambda e: e.tensor_tensor(out=pos3[:, 1:, :], in0=lp3[:, 1:, :], in1=incl3[:, :NB - 1, :], op=ALU.add), extra_r=[p1b])
        n8 = incl3[:, NB - 1, :]
        thr = p2s[:, 0:NT]
        ns8 = p2s[:, 32:40]
        bi8 = p2s[:, 40:48]
        ends8 = p2s[:, 48:56]
        base8 = p2s[:, 56:64]
        sthr = p2s[:, 64:64 + NSLOT]
        es = p2s[:, 112:112 + NSLOT]
        jthr = p2s[:, 160:182]
        pcol = p2s[:, 182:183]
        wb = p2s[:, 184:184 + NSLOT]
        fw.op("pool", lambda e: e.iota(thr, pattern=[[512, NT]], base=0, channel_multiplier=0,
                                       allow_small_or_imprecise_dtypes=True), writes=[P2])
        fw.op("pool", lambda e: e.iota(sthr, pattern=[[512, NSLOT]], base=0, channel_multiplier=0,
                                       allow_small_or_imprecise_dtypes=True), writes=[P2])
        fw.op("pool", lambda e: e.iota(jthr, pattern=[[128, 22]], base=0, channel_multiplier=0,
                                       allow_small_or_imprecise_dtypes=True), writes=[P2])
        fw.op("pool", lambda e: e.iota(pcol, pattern=[[0, 1]], base=NBLK1 * 128, channel_multiplier=1,
                                       allow_small_or_imprecise_dtypes=True), writes=[P2])
        cmp1 = v3(prod[:, 0:8 * NT], NT)
        p2op(lambda e: e.tensor_tensor(out=cmp1, in0=n8.unsqueeze(2).to_broadcast([128, 8, NT]),
                                       in1=thr.unsqueeze(1).to_broadcast([128, 8, NT]), op=ALU.is_gt))
        p2op(lambda e: e.tensor_reduce(out=ns8, in_=cmp1, axis=AX.X, op=ALU.add))
        p2op(lambda e: e.tensor_copy(out=bi8[:, 0:1], in_=ns8[:, 0:1]))
        for e_ in range(1, NE):
            p2op((lambda e_i: lambda e: e.tensor_tensor(out=bi8[:, e_i:e_i + 1], in0=bi8[:, e_i - 1:e_i], in1=ns8[:, e_i:e_i + 1], op=ALU.add))(e_))
        p2op(lambda e: e.tensor_scalar(out=ends8, in0=bi8, scalar1=512.0, scalar2=None, op0=ALU.mult))
        p2op(lambda e: e.scalar_tensor_tensor(out=base8, in0=ns8, scalar=-512.0, in1=ends8, op0=ALU.mult, op1=ALU.add))
        dst3 = v3(dstf, 8)
        p2op(lambda e: e.tensor_tensor(out=dst3, in0=pos3, in1=base8.unsqueeze(1).to_broadcast([128, NB, 8]), op=ALU.add))
        p2op(lambda e: e.tensor_tensor(out=m2f, in0=Mf, in1=s1A, op=ALU.subtract))
        tabi = tab_all.bitcast(I32)
        idx1i = tabi[:, 0:NB]
        idx2i = tabi[:, 64:64 + NB]
        g1 = tab_all[:, 128:128 + NB]
        g2 = tab_all[:, 192:192 + NB]
        widxi = tabi[:, 256:256 + NSLOT * 22]
        assert 256 + NSLOT * 22 <= 2048
        i1f = csA[:, 0:NB]
        i2f = csB[:, 0:NB]
        prod3 = v3(prod, 8)
        p2op(lambda e: e.tensor_tensor(out=prod, in0=s1A, in1=dstf, op=ALU.mult))
        p2op(lambda e: e.tensor_reduce(out=i1f, in_=prod3, axis=AX.X, op=ALU.add))
        p2op(lambda e: e.tensor_copy(out=idx1i, in_=i1f), extra_w=[tabb])
        p2op(lambda e: e.tensor_tensor(out=prod, in0=m2f, in1=dstf, op=ALU.mult))
        p2op(lambda e: e.tensor_reduce(out=i2f, in_=prod3, axis=AX.X, op=ALU.add))
        p2op(lambda e: e.tensor_copy(out=idx2i, in_=i2f), extra_w=[tabb])
        p2op(lambda e: e.tensor_tensor(out=prod, in0=s1A, in1=gA, op=ALU.mult))
        p2op(lambda e: e.tensor_reduce(out=g1, in_=prod3, axis=AX.X, op=ALU.add), extra_w=[tabb])
        p2op(lambda e: e.tensor_tensor(out=prod, in0=m2f, in1=gA, op=ALU.mult))
        p2op(lambda e: e.tensor_reduce(out=g2, in_=prod3, axis=AX.X, op=ALU.add), extra_w=[tabb])
        cmp2 = v3(Mf[:, 0:NSLOT * 8] if NSLOT * 8 <= W8 else tmp_all[:, 0:NSLOT * 8], 8)
        p2op(lambda e: e.tensor_tensor(out=cmp2, in0=ends8.unsqueeze(1).to_broadcast([128, NSLOT, 8]),
                                       in1=sthr.unsqueeze(2).to_broadcast([128, NSLOT, 8]), op=ALU.is_le))
        p2op(lambda e: e.tensor_reduce(out=es, in_=cmp2, axis=AX.X, op=ALU.add))
        p2op(lambda e: e.tensor_scalar(out=es, in0=es, scalar1=float(NE - 1), scalar2=None, op0=ALU.min))
        p2op(lambda e: e.tensor_scalar(out=wb, in0=es, scalar1=float(22 * 128), scalar2=pcol, op0=ALU.mult, op1=ALU.add))
        widxf = v3(tmp_all[:, 1024:1024 + NSLOT * 22], 22)
        p2op(lambda e: e.tensor_tensor(out=widxf, in0=wb.unsqueeze(2).to_broadcast([128, NSLOT, 22]),
                                       in1=jthr.unsqueeze(1).to_broadcast([128, NSLOT, 22]), op=ALU.add))
        p2op(lambda e: e.tensor_copy(out=widxi, in_=tmp_all[:, 1024:1024 + NSLOT * 22]), extra_w=[tabb])
        widx_ref[0] = widxi
        widx_ref[1] = tabb
        barrier[0] = len(seq)

        for bg in range(NB):
            ta, tb_, ti = ntmp()
            stg = ta.bitcast(BF16)
            dmas("sp", f"tm{ti}", stg, xn_scr[bg * 128:(bg + 1) * 128, :], [xnsb], [tb_])
            for idx in (idx1i, idx2i):
                fw.dma("pool", f"sc{ti}", (lambda i_, b_, s_: lambda e: e.indirect_dma_start(
                    out=xg[:, :], out_offset=bass.IndirectOffsetOnAxis(ap=i_[:, b_:b_ + 1], axis=0),
                    in_=s_, in_offset=None))(idx, bg, stg),
                    reads=[tb_, tabb, xgz], writes=[xgs])

        def evac_y(dc, pa, pb):
            if dc % 2 == 0:
                act(hT[:, dc, :], pa, AF.Copy, reads=[pb], writes=[hTb[dc]])
            else:
                cp("dve", hT[:, dc, :], pa, [pb], [hTb[dc]])

        def load_xs(s_):
            dmas("sp", "xs", xs3, xg[s_ * 512:(s_ + 1) * 512, :].rearrange("(tb p) d -> p tb d", p=128), [xgz, xgs], [KS])

        load_xs(0)
        for s in range(NSLOT):
            for kp in range(4):
                pa, pb = nps()
                pab = pa.bitcast(BF16)
                for k2 in range(2):
                    k = kp * 2 + k2
                    for tb in range(4):
                        tr(pab[:, k2 * 512 + tb * 128:k2 * 512 + (tb + 1) * 128], pb, xs3[:, tb, k * 128:(k + 1) * 128],
                           identb, [KS, identbb])
                if kp % 2 == 0:
                    act(xnT[:, kp * 2:kp * 2 + 2, :], v3(pab, 512), AF.Copy, reads=[pb], writes=[xnTb[kp * 2], xnTb[kp * 2 + 1]])
                else:
                    cp("dve", xnT[:, kp * 2:kp * 2 + 2, :], v3(pab, 512), [pb], [xnTb[kp * 2], xnTb[kp * 2 + 1]])
            if s + 1 < NSLOT:
                load_xs(s + 1)
            ffn(lambda j: fetch(("M", s, j)), evac_y)
            store_tokmajor_f32(hT, lambda k: [hTb[k]], lambda tb: yg[s * 512 + tb * 128:s * 512 + (tb + 1) * 128, :], ygb, "sp")

        gfin = hT_all[:, 0:1024]
        dmas("sp", "gf", gfin, gfin_d[:, :], [], [hTb[0], hTb[1]])
        P4 = []
        regs = [(va_f, [VA]), (vb_f, [VB]),
                (arena[:, wsl_off:wsl_off + 4096], [wslb[0], wslb[1]]),
                (arena[:, wsl_off + 4096:wsl_off + 8192], [wslb[2], wslb[3]])]
        NSET = len(regs)
        for si, (reg, rb) in enumerate(regs):
            P4.append([(reg[:, i * 1024:(i + 1) * 1024], Buf(f"p4_{si}_{i}")) for i in range(4)])
        first = [True] * NSET
        exw = {}

        def p4_loads(bg):
            si = bg % NSET
            (h3a, h3bf), (y1a, y1b), (y2a, y2b), (oa, ob) = P4[si]
            ex_w = list(regs[si][1]) if first[si] else []
            first[si] = False
            exw[bg] = ex_w
            dmas("sp", f"a{si}0", h3a, h3_scr[bg * 128:(bg + 1) * 128, :], [h3b], [h3bf] + ex_w)
            for (ya, yb_, idx, key) in ((y1a, y1b, idx1i, f"a{si}1"), (y2a, y2b, idx2i, f"a{si}2")):
                fw.dma("pool", key, (lambda o_, i_, b_: lambda e: e.indirect_dma_start(
                    out=o_, out_offset=None, in_=yg[:, :],
                    in_offset=bass.IndirectOffsetOnAxis(ap=i_[:, b_:b_ + 1], axis=0)))(ya, idx, bg),
                    reads=[ygb, tabb], writes=[yb_] + ex_w)

        for bg in range(min(NSET - 1, NB)):
            p4_loads(bg)
        for bg in range(NB):
            if bg + NSET - 1 < NB:
                p4_loads(bg + NSET - 1)
            si = bg % NSET
            (h3a, h3bf), (y1a, y1b), (y2a, y2b), (oa, ob) = P4[si]
            ex_w = exw[bg]
            sq_ = ssq4[:, (bg % 4) * 4:(bg % 4) * 4 + 4]
            sqb_ = ssqhb[bg % 4]
            stt(h3a, y1a, g1[:, bg:bg + 1], h3a, ALU.mult, ALU.add, [y1b, tabb, h3bf], [h3bf])
            stt(h3a, y2a, g2[:, bg:bg + 1], h3a, ALU.mult, ALU.add, [y2b, tabb, h3bf], [h3bf])
            act(y1a, h3a, AF.Square, reads=[h3bf], writes=[y1b, sqb_], accum=sq_[:, 0:1])
            act(sq_[:, 1:2], sq_[:, 0:1], AF.Sqrt, reads=[sqb_, epsbb], writes=[sqb_], scale=1.0 / D, bias=epsb[:, 0:1])
            fw.op("dve", (lambda q_: lambda e: e.reciprocal(out=q_[:, 2:3], in_=q_[:, 1:2]))(sq_), reads=[sqb_], writes=[sqb_])
            stt(oa, h3a, sq_[:, 2:3], gfin, ALU.mult, ALU.mult, [h3bf, sqb_, hTb[0], hTb[1]], [ob] + ex_w)
            dmas("sp", f"a{si}3", out[bg * 128:(bg + 1) * 128, :], oa, [ob], [outb])

        fw.fence("sp", [P4[i][3][1] for i in range(NSET)] + wslb + iob + [outb])
        if dbg:
            fw.fence("sp", hTb)
        fw.emit_all(st)
        build_nc.stats = {e: len(fw.ops[e]) for e in ENGS}
        build_nc.stats["waits"] = fw.n_waits
    return nc


def host_consts(S):
    consts = np.zeros((128, 520), np.float64)
    j = np.arange(128)[:, None].astype(np.float64)
    i = np.arange(128)[None, :].astype(np.float64)
    for h in range(H):
        g = GAM[h]
        consts[:, h * 128:(h + 1) * 128] = np.where(i >= j, g ** (-(j + 1.0)), 0.0)
        consts[:, 512 + h] = g ** (np.arange(128) + 1.0)
        consts[:, 516 + h] = g ** (127.0 - np.arange(128))
    inv_freq = (np.float32(10000.0) ** (-(np.arange(0, 256, 2, dtype=np.float32)) / np.float32(256))).astype(np.float32)
    pos = np.arange(S, dtype=np.float32)
    ang = (inv_freq[:, None] * pos[None, :]).astype(np.float32).astype(np.float64)
    tabs = np.stack([np.cos(ang), np.sin(ang), np.cos(ang) / 16.0, np.sin(ang) / 16.0]).astype(np.float32)
    return consts.astype(np.float32), tabs


def host_small(inputs):
    small = np.zeros((128, 128), np.float32)
    for n, name in enumerate(["norm_mix0", "norm_ffn0", "norm_mix1", "norm_ffn1", "norm_final"]):
        small[:, n * 8:(n + 1) * 8] = np.asarray(inputs[name], np.float32).reshape(8, 128).T
    cw = np.asarray(inputs["conv_w"], np.float32)
    for jj in range(3):
        small[:, 40 + jj * 8:48 + jj * 8] = cw[jj].reshape(8, 128).T
    wr = np.asarray(inputs["moe_router"], np.float32)
    small[:, 64:128] = wr.reshape(8, 128, 8).transpose(1, 0, 2).reshape(128, 64)
    return small


WNAMES = ["ret_w_in", "ret_w_out", "ffn_w_gate", "ffn_w_up", "ffn_w_down", "conv_w_in", "conv_w_out",
          "moe_w_gate", "moe_w_up", "moe_w_down"]


def make_in_maps(inputs):
    x = np.asarray(inputs["x"], np.float32)
    B, S, _ = x.shape
    consts, tabs = host_consts(S)
    small = host_small(inputs)
    common = {n: np.ascontiguousarray(np.asarray(inputs[n], np.float32)) for n in WNAMES}
    common["small"] = small
    common["consts"] = consts
    common["tabs"] = tabs
    common["gfin"] = np.ascontiguousarray(np.broadcast_to(np.asarray(inputs["norm_final"], np.float32)[None, :], (128, D)))
    return [dict(common, x=np.ascontiguousarray(x[b])) for b in range(B)], B, S


def kernel(**inputs):
    in_maps, B, S = make_in_maps(inputs)
    nc = build_nc(S)
    res = run_bass_kernel_spmd(nc, in_maps, core_ids=list(range(B)))
    return np.stack([np.asarray(r["out"], np.float32) for r in res.results]).astype(np.float32)
```

```python
import contextlib
import numpy as np
import concourse.bass as bass
import concourse.mybir as mybir
from concourse.bass_utils import run_bass_kernel_spmd

F32 = mybir.dt.float32
BF16 = mybir.dt.bfloat16
AF = mybir.ActivationFunctionType
ALU = mybir.AluOpType
AX = mybir.AxisListType

ENGS = ("sp", "act", "dve", "pool", "pe")


class Buf:
    __slots__ = ("name", "ap", "writers", "readers", "disjoint")

    def __init__(self, name, ap=None, disjoint=False):
        self.name = name
        self.ap = ap
        self.disjoint = disjoint
        self.writers = {}
        self.readers = {}


class Op:
    __slots__ = ("emit", "deps", "marked", "dma")

    def __init__(self, emit, deps, dma):
        self.emit = emit
        self.deps = deps
        self.marked = False
        self.dma = dma


class FW:
    def __init__(self, nc):
        self.nc = nc
        self.ops = {e: [] for e in ENGS}
        self.dma_count = {}
        self.n_waits = 0

    def _collect(self, eng, reads, writes):
        deps = {}

        def add(tok):
            cls = (tok[0], tok[1])
            if deps.get(cls, -1) < tok[2]:
                deps[cls] = tok[2]

        for b in reads:
            for tok in b.writers.values():
                if tok[0] == "eng" and tok[1] == "pe" and eng == "pe":
                    continue
                add(tok)
        for b in writes:
            for d in (b.writers, b.readers):
                for tok in d.values():
                    if tok[0] == "eng" and tok[1] == eng:
                        continue
                    if d is b.writers and b.disjoint and tok[0] == "dma" and eng.startswith("dma:"):
                        continue
                    add(tok)
        return deps

    def _mark(self, deps):
        for (kind, k), v in deps.items():
            if kind == "eng":
                self.ops[k][v].marked = True

    def op(self, eng, emit, reads=(), writes=()):
        deps = self._collect(eng, reads, writes)
        self._mark(deps)
        idx = len(self.ops[eng])
        self.ops[eng].append(Op(emit, deps, None))
        tok = ("eng", eng, idx)
        for b in reads:
            b.readers[eng] = tok
        for b in writes:
            b.writers[eng] = tok

    def dma(self, eng, key, emit, reads=(), writes=()):
        cls = "dma:" + key
        deps = self._collect(cls, reads, writes)
        self._mark(deps)
        val = self.dma_count.get(key, 0) + 16
        self.dma_count[key] = val
        self.ops[eng].append(Op(emit, deps, (key, val)))
        tok = ("dma", key, val)
        for b in reads:
            b.readers[cls] = tok
        for b in writes:
            b.writers[cls] = tok

    def fence(self, eng, bufs):
        deps = self._collect(eng + "_fence", list(bufs), list(bufs))
        self._mark(deps)
        self.ops[eng].append(Op(None, deps, None))

    def emit_all(self, stack):
        nc = self.nc
        sems = {e: stack.enter_context(nc.semaphore("s_" + e)) for e in ENGS if e != "sp"}
        dsems = {k: stack.enter_context(nc.semaphore("d_" + k)) for k in self.dma_count}
        counts = {}
        for e in ENGS:
            c = 0
            arr = []
            for o in self.ops[e]:
                if o.marked:
                    c += 1
                arr.append(c)
            counts[e] = arr
        block = stack.enter_context(nc.Block())
        fw = self

        def run(e, engobj):
            water = {}
            for o in fw.ops[e]:
                for (kind, k), v in o.deps.items():
                    if kind == "eng":
                        sem = sems[k]
                        val = counts[k][v]
                    else:
                        sem = dsems[k]
                        val = v
                    wk = (kind, k)
                    if water.get(wk, 0) < val:
                        engobj.wait_ge(sem, val)
                        water[wk] = val
                        fw.n_waits += 1
                if o.emit is None:
                    continue
                inst = o.emit(engobj)
                if o.dma is not None:
                    inst.then_inc(dsems[o.dma[0]], 16)
                elif o.marked:
                    inst.then_inc(sems[e], 1)

        @block.sync
        def _(eng):
            run("sp", eng)

        @block.scalar
        def _(eng):
            run("act", eng)

        @block.vector
        def _(eng):
            run("dve", eng)

        @block.gpsimd
        def _(eng):
            run("pool", eng)

        @block.tensor
        def _(eng):
            run("pe", eng)


I32 = mybir.dt.int32
D = 1024
H = 4
FF = 3584
NE = 8
TT = 512
EPS = 1e-6
NW = 4
LA = NW - 2
GAM = [1.0 - 2.0 ** (-5.0 - h) for h in range(H)]


def build_nc(S, dbg=False):
    NT = S // TT
    NB = NT * 4
    NSLOT = (2 * S) // 512 + NE
    nc = bass.Bass("TRN2", target_bir_lowering=False)

    def din(name, shape, dt=F32):
        return nc.dram_tensor(name, list(shape), dt, kind="ExternalInput").ap()

    x = din("x", [S, D])
    ret_w_in = din("ret_w_in", [D, 6144])
    ret_w_out = din("ret_w_out", [2048, D])
    ffn_w_gate = din("ffn_w_gate", [D, FF])
    ffn_w_up = din("ffn_w_up", [D, FF])
    ffn_w_down = din("ffn_w_down", [FF, D])
    conv_w_in = din("conv_w_in", [D, 3072])
    conv_w_out = din("conv_w_out", [D, D])
    moe_w_gate = din("moe_w_gate", [NE, D, FF])
    moe_w_up = din("moe_w_up", [NE, D, FF])
    moe_w_down = din("moe_w_down", [NE, FF, D])
    small_d = din("small", [128, 128])
    consts_d = din("consts", [128, 520])
    tabs_d = din("tabs", [4, 128, S])
    gfin_d = din("gfin", [128, D])
    out = nc.dram_tensor("out", [S, D], F32, kind="ExternalOutput").ap()
    if dbg:
        dbg_d = nc.dram_tensor("dbg", [4, 8, 128, S], F32, kind="ExternalOutput").ap()

    blocks = []

    def addA(w, c0):
        blocks.append(("A", w, c0))
        return len(blocks) - 1

    def addB(w, c0):
        blocks.append(("B", w, c0))
        return len(blocks) - 1

    def addC(w, c0):
        blocks.append(("C", w, c0))
        return len(blocks) - 1

    bl_qk = [addA(ret_w_in, c * 512) for c in range(4)]
    bl_v = [addA(ret_w_in, 2048 + c * 512) for c in range(4)]
    bl_g = [addA(ret_w_in, 4096 + c * 512) for c in range(4)]
    bl_wo = [addB(ret_w_out, c * 256) for c in range(4)]
    bl_ffn = []
    for fb in range(7):
        bl_ffn.append(addA(ffn_w_gate, fb * 512))
        bl_ffn.append(addA(ffn_w_up, fb * 512))
    for dc in range(8):
        bl_ffn.append(addC(ffn_w_down, dc * 128))
    bl_conv = []
    for half in range(2):
        bl_conv.append((addA(conv_w_in, 1024 + half * 512), addA(conv_w_in, 2048 + half * 512),
                        addA(conv_w_in, half * 512)))
    bl_co = [addA(conv_w_out, c * 512) for c in range(2)]
    NBLK1 = len(blocks)
    moe_blocks = []
    for e in range(NE):
        for fb in range(7):
            moe_blocks.append(("A", moe_w_gate[e], fb * 512))
            moe_blocks.append(("A", moe_w_up[e], fb * 512))
        for dc in range(8):
            moe_blocks.append(("C", moe_w_down[e], dc * 128))
    NROWS = (NBLK1 + len(moe_blocks)) * 128
    wscr = nc.dram_tensor("wscr", [NROWS, 4096], BF16, kind="Internal").ap()
    h3_scr = nc.dram_tensor("h3_scr", [S, D], F32, kind="Internal").ap()
    xn_scr = nc.dram_tensor("xn_scr", [S, D], BF16, kind="Internal").ap()
    xg = nc.dram_tensor("xg", [NSLOT * 512, D], BF16, kind="Internal").ap()
    yg = nc.dram_tensor("yg", [NSLOT * 512, D], F32, kind="Internal").ap()
    scrb = [Buf(f"scr{i}") for i in range(NBLK1)]
    moescrb = Buf("moescr")
    h3b = Buf("h3scr")
    xnsb = Buf("xnscr")
    xgz = Buf("xgz", disjoint=True)
    xgs = Buf("xgs", disjoint=True)
    ygb = Buf("yg")
    outb = Buf("outdram", disjoint=True)

    st = contextlib.ExitStack()
    with st:
        fw = FW(nc)
        ARENA = 47800
        arena = st.enter_context(nc.sbuf_tensor("arena", [128, ARENA], F32))
        off = [0]

        def carve(n):
            a = arena[:, off[0]:off[0] + n]
            off[0] += n
            assert off[0] <= ARENA, off[0]
            return a

        def v3(ap, b):
            return ap.rearrange("p (a b) -> p a b", b=b)

        hT_all = carve(4096)
        hT = v3(hT_all, 512)
        hTb = [Buf(f"hT{k}") for k in range(8)]
        xnT_all = carve(2048).bitcast(BF16)
        xnT = v3(xnT_all, 512)
        xnTb = [Buf(f"xnT{k}") for k in range(8)]
        io_ap = [carve(1024), carve(1024)]
        iob = [Buf("io0"), Buf("io1")]
        tab_all = carve(2048)
        tab = v3(tab_all, 512)
        tabb = Buf("tab")
        qk_f = carve(4096)
        xin = v3(qk_f, 1024)
        qk_all = qk_f.bitcast(BF16)
        qT = v3(qk_all[:, 0:4096], 512)
        kT = v3(qk_all[:, 4096:8192], 512)
        ogT = v3(qk_all, 512)
        yT = qT
        QA = Buf("QA")
        QB = Buf("QB")
        ks_all = carve(2048).bitcast(BF16)
        ktok = v3(ks_all, 1024)
        sq_all = ks_all
        sq = v3(ks_all, 512)
        xs3 = ktok
        KS = Buf("KS")
        va_f = carve(4224)
        VA = Buf("VA")
        vtok = v3(va_f[:, 0:4096].bitcast(BF16), 2048)
        uT = v3(va_f[:, 0:4112], 514)
        xnf = uT[:, :, 2:514]
        hffA = v3(va_f[:, 0:4096].bitcast(BF16), 512)
        vb_f = carve(4096)
        VB = Buf("VB")
        gtok = v3(vb_f.bitcast(BF16), 2048)
        hffB = v3(vb_f.bitcast(BF16), 512)
        stF = [v3(carve(1024), 512) for _ in range(H)]
        stFb = [[Buf(f"stF{h}_{dc}") for dc in range(2)] for h in range(H)]
        stB = [v3(carve(512).bitcast(BF16), 512) for _ in range(H)]
        stBb = [[Buf(f"stB{h}_{dc}") for dc in range(2)] for h in range(H)]
        halo = v3(carve(16), 2)
        halob = Buf("halo")
        wsl_off = off[0]
        wsl = [carve(2048).bitcast(BF16) for _ in range(NW)]
        wslb = [Buf(f"ws{i}") for i in range(NW)]
        NTMP = 6
        tmp_all = carve(512 * NTMP)
        tmp_ap = [tmp_all[:, i * 512:(i + 1) * 512] for i in range(NTMP)]
        tmpb = [Buf(f"tmp{i}") for i in range(NTMP)]
        rstd = carve(512)
        rstdb = Buf("rstd")
        consts = carve(520)
        constb = Buf("consts")
        small = carve(128)
        smallb = Buf("small")
        identf = carve(128)
        identfb = Buf("identf")
        iot = carve(128)
        identb = carve(64).bitcast(BF16)
        identbb = Buf("identb")
        onesb = carve(64).bitcast(BF16)
        onesbb = Buf("onesb")
        lstrb = carve(64).bitcast(BF16)
        lstrbb = Buf("lstr")
        epsb = carve(1)
        epsbb = Buf("eps")
        sT_ap = [carve(256).bitcast(BF16), carve(256).bitcast(BF16)]
        sTb = [Buf("sT0"), Buf("sT1")]
        ssq4 = carve(16)
        ssqhb = [Buf(f"ssqh{h}") for h in range(H)]
        junk = carve(256).bitcast(BF16)
        junkb = Buf("junk")
        ssq = carve(4)
        ssqb = Buf("ssq")
        lg = carve(32)
        lgb = Buf("lg")
        m8 = carve(32)
        m8b = Buf("m8")
        rt = carve(64)
        rtb = Buf("rt")
        gA = carve(NB * 8)
        gAb = Buf("gA")
        s1A = carve(NB * 8)
        s1b = Buf("s1A")
        p2s = carve(256)
        P2 = Buf("P2")
        zt = carve(512).bitcast(BF16)
        ztb = Buf("zt")

        PS = []
        for i in range(8):
            PS.append((st.enter_context(nc.psum_tensor(f"ps{i}", [128, 512], F32))[:, :], Buf(f"ps{i}")))
        psi = [0]

        def nps():
            p = PS[psi[0] % 8]
            psi[0] += 1
            return p

        tmi = [0]

        def ntmp():
            i = tmi[0] % NTMP
            tmi[0] += 1
            return tmp_ap[i], tmpb[i], i

        Mt = consts[:, 0:512]
        qdec = consts[:, 512:516]
        kdec = consts[:, 516:520]

        def mm(psap, psbuf, lhsT, rhs, start, stop, reads):
            fw.op("pe", lambda e: e.matmul(psap, lhsT=lhsT, rhs=rhs, start=start, stop=stop),
                  reads=reads, writes=[psbuf])

        def tr(psap, psbuf, in_, ident, reads):
            fw.op("pe", lambda e: e.transpose(psap, in_=in_, identity=ident), reads=reads, writes=[psbuf])

        def act(out_, in_, func, reads, writes, scale=None, bias=None, accum=None):
            kw = {}
            if scale is not None:
                kw["scale"] = scale
            if bias is not None:
                kw["bias"] = bias
            if accum is not None:
                kw["accum_out"] = accum
            fw.op("act", lambda e: e.activation(out=out_, in_=in_, func=func, **kw), reads=reads, writes=writes)

        def tt(eng, out_, in0, in1, op, reads, writes):
            fw.op(eng, lambda e: e.tensor_tensor(out=out_, in0=in0, in1=in1, op=op), reads=reads, writes=writes)

        def stt(out_, in0, scalar, in1, op0, op1, reads, writes):
            fw.op("dve", lambda e: e.scalar_tensor_tensor(out=out_, in0=in0, scalar=scalar, in1=in1, op0=op0, op1=op1),
                  reads=reads, writes=writes)

        def ts(eng, out_, in0, s1, op0, reads, writes, s2=None, op1=None):
            if op1 is None:
                fw.op(eng, lambda e: e.tensor_scalar(out=out_, in0=in0, scalar1=s1, scalar2=None, op0=op0),
                      reads=reads, writes=writes)
            else:
                fw.op(eng, lambda e: e.tensor_scalar(out=out_, in0=in0, scalar1=s1, scalar2=s2, op0=op0, op1=op1),
                      reads=reads, writes=writes)

        def cp(eng, out_, in_, reads, writes):
            fw.op(eng, lambda e: e.tensor_copy(out=out_, in_=in_), reads=reads, writes=writes)

        def red(out_, in_, reads, writes):
            fw.op("dve", lambda e: e.tensor_reduce(out=out_, in_=in_, axis=AX.X, op=ALU.add), reads=reads, writes=writes)

        def dmas(eng, key, out_, in_, reads, writes):
            fw.dma(eng, key, lambda e: e.dma_start(out=out_, in_=in_), reads=reads, writes=writes)

        dmas("sp", "cst", consts, consts_d[:, :], [], [constb])
        dmas("sp", "cst2", small, small_d[:, :], [], [smallb])
        iotb = Buf("iot")
        fw.op("pool", lambda e: e.iota(iot, pattern=[[1, 128]], base=0, channel_multiplier=-1,
                                       allow_small_or_imprecise_dtypes=True), writes=[iotb])
        fw.op("dve", lambda e: e.tensor_single_scalar(out=identf, in_=iot, scalar=0.0, op=ALU.is_equal),
              reads=[iotb], writes=[identfb])
        fw.op("dve", lambda e: e.tensor_single_scalar(out=identb, in_=iot, scalar=0.0, op=ALU.is_equal),
              reads=[iotb], writes=[identbb])
        fw.op("dve", lambda e: e.tensor_single_scalar(out=lstrb, in_=iot, scalar=0.0, op=ALU.is_gt),
              reads=[iotb], writes=[lstrbb])
        fw.op("dve", lambda e: e.memset(onesb, 1.0), writes=[onesbb])
        fw.op("dve", lambda e: e.memset(epsb, EPS), writes=[epsbb])
        for h in range(H):
            fw.op("pool", (lambda hh: lambda e: e.memset(stF[hh], 0.0))(h), writes=stFb[h])
            fw.op("pool", (lambda hh: lambda e: e.memset(stB[hh], 0.0))(h), writes=stBb[h])
        fw.op("pool", lambda e: e.memset(halo, 0.0), writes=[halob])
        fw.op("pool", lambda e: e.memset(zt, 0.0), writes=[ztb])

        seq = [("R", t, bi) for t in range(NT) for bi in range(NBLK1)]
        N1 = len(seq)
        seq += [("M", s, j) for s in range(NSLOT) for j in range(22)]
        issued = [0]
        barrier = [N1]
        widx_ref = [None, None]

        def blk_n(kind):
            return 4096 if kind in ("A", "B") else 3584

        def src_view(kind, w, c0):
            if kind == "A":
                return w[:, c0:c0 + 512].rearrange("(k p) j -> p k j", p=128), 512
            if kind == "B":
                return w[:, c0:c0 + 256].rearrange("(k p) j -> p k j", p=128), 256
            return w[:, c0:c0 + 128].rearrange("(k p) j -> p k j", p=128), 128

        def issue_load(g):
            ent = seq[g]
            si = g % NW
            slot = wsl[si]
            sb = wslb[si]
            if ent[0] == "M":
                _, s, j = ent
                col = s * 22 + j
                widx, widxb = widx_ref
                fw.dma("pool", f"wp{si}", lambda e: e.indirect_dma_start(
                    out=slot, out_offset=None, in_=wscr[:, :],
                    in_offset=bass.IndirectOffsetOnAxis(ap=widx[:, col:col + 1], axis=0)), reads=[moescrb, widxb], writes=[sb])
                return
            _, t, bi = ent
            kind, w, c0 = blocks[bi]
            n = blk_n(kind)
            rows = wscr[bi * 128:(bi + 1) * 128, 0:n]
            if t == 0:
                src, jw = src_view(kind, w, c0)
                dst = v3(slot[:, 0:n], jw)
                if kind == "C":
                    dmas("pool", f"wp{si}", dst[:, 0:14, :], src[:, 0:14, :], [], [sb])
                    dmas("pool", f"wp{si}", dst[:, 14:28, :], src[:, 14:28, :], [], [sb])
                else:
                    dmas("pool", f"wp{si}", dst, src, [], [sb])
                if NT > 1:
                    dmas("sp", f"s{si}", rows, slot[:, 0:n], [sb], [scrb[bi]])
            else:
                dmas("sp", f"w{si}", slot[:, 0:n], rows, [scrb[bi]], [sb])

        gctr = [0]

        def fetch(key):
            g = gctr[0]
            assert seq[g] == key, (seq[g], key)
            gctr[0] += 1
            while issued[0] <= min(g + LA, barrier[0] - 1):
                issue_load(issued[0])
                issued[0] += 1
            si = g % NW
            return wsl[si], wslb[si]

        cv_next = [0]
        cv_per_fetch = -(-len(moe_blocks) // max(1, (NT - 1) * NBLK1))
        cv_stride = max(1, ((NT - 1) * NBLK1) // len(moe_blocks))
        cv_tick = [0]

        def cv_some(n):
            for _ in range(n):
                i = cv_next[0]
                if i >= len(moe_blocks):
                    return
                cv_next[0] += 1
                kind, w, c0 = moe_blocks[i]
                src, jw = src_view(kind, w, c0)
                nn = blk_n(kind)
                r0 = (NBLK1 + i) * 128
                dst = wscr[r0:r0 + 128, 0:nn].rearrange("p (k j) -> p k j", j=jw)
                if kind == "C":
                    dmas("pool", "cv", dst[:, 0:14, :], src[:, 0:14, :], [], [moescrb])
                    dmas("pool", "cv", dst[:, 14:28, :], src[:, 14:28, :], [], [moescrb])
                else:
                    dmas("pool", "cv", dst, src, [], [moescrb])

        zf_next = [0]

        def zf_some(n):
            for _ in range(n):
                r = zf_next[0]
                if r >= NSLOT * 4:
                    return
                zf_next[0] += 1
                dmas("sp", "zf", xg[r * 128:(r + 1) * 128, :], zt, [ztb], [xgz])

        def fetch1(t, bi):
            if t >= 1 or NT == 1:
                cv_tick[0] += 1
                if cv_tick[0] % cv_stride == 0:
                    cv_some(cv_per_fetch)
                zf_some(1 if NT > 4 else 8)
            return fetch(("R", t, bi))

        def hff(f):
            return (hffA[:, f, :], VA) if f < 16 else (hffB[:, f - 16, :], VB)

        def sq_chunk(k):
            act(sq[:, k, :], hT[:, k, :], AF.Square, reads=[hTb[k]], writes=[KS])

        def rmsnorm(nidx, f32out=False, presq=True):
            if not presq:
                act(sq_all, hT_all, AF.Square, reads=hTb, writes=[KS])
            pa, pb = nps()
            for k in range(8):
                mm(pa, pb, onesb, sq[:, k, :], k == 0, k == 7, [onesbb, KS])
            act(rstd, pa, AF.Ln, reads=[pb, epsbb], writes=[rstdb], scale=1.0 / D, bias=epsb[:, 0:1])
            act(rstd, rstd, AF.Exp, reads=[rstdb], writes=[rstdb], scale=-0.5)
            for k in range(8):
                g = small[:, nidx * 8 + k:nidx * 8 + k + 1]
                if f32out:
                    stt(xnf[:, k, :], hT[:, k, :], g, rstd, ALU.mult, ALU.mult, [hTb[k], smallb, rstdb], [VA])
                else:
                    stt(xnT[:, k, :], hT[:, k, :], g, rstd, ALU.mult, ALU.mult, [hTb[k], smallb, rstdb], [xnTb[k]])

        def dump(stage, t):
            if dbg:
                dst = dbg_d[stage, :, :, t * TT:(t + 1) * TT].rearrange("k p s -> p k s")
                dmas("pool", "dbg", dst, hT, hTb, [])

        def resid_add(dchunk, pa, pb):
            tt("dve", hT[:, dchunk, :], pa, hT[:, dchunk, :], ALU.add, [pb, hTb[dchunk]], [hTb[dchunk]])
            sq_chunk(dchunk)

        def ffn(fetchj, evac):
            for fb in range(7):
                sg, sgb = fetchj(2 * fb)
                su, sub = fetchj(2 * fb + 1)
                sg3 = v3(sg, 512)
                su3 = v3(su, 512)
                for fc in range(4):
                    f = fb * 4 + fc
                    pga, pgb = nps()
                    pua, pub = nps()
                    for k in range(8):
                        mm(pga, pgb, sg3[:, k, fc * 128:(fc + 1) * 128], xnT[:, k, :], k == 0, k == 7, [sgb, xnTb[k]])
                    for k in range(8):
                        mm(pua, pub, su3[:, k, fc * 128:(fc + 1) * 128], xnT[:, k, :], k == 0, k == 7, [sub, xnTb[k]])
                    ta, tb_, _ = ntmp()
                    act(ta, pga, AF.Silu, reads=[pgb], writes=[tb_])
                    ha, hb = hff(f)
                    tt("dve", ha, pua, ta, ALU.mult, [pub, tb_], [hb])
            for dc in range(8):
                sd, sdb = fetchj(14 + dc)
                sd3 = v3(sd[:, 0:3584], 128)
                pa, pb = nps()
                for f in range(28):
                    ha, hb = hff(f)
                    mm(pa, pb, sd3[:, f, :], ha, f == 0, f == 27, [sdb, hb])
                evac(dc, pa, pb)

        def store_tokmajor_f32(srcT, src_reads, dram_rows, dram_buf, queue):
            for tb in range(4):
                io = io_ap[tb % 2]
                ib = iob[tb % 2]
                for half in range(2):
                    pa, pb = nps()
                    for kk in range(4):
                        k = half * 4 + kk
                        tr(pa[:, kk * 128:(kk + 1) * 128], pb, srcT[:, k, tb * 128:(tb + 1) * 128], identf,
                           src_reads(k) + [identfb])
                    if half == 0:
                        act(io[:, 0:512], pa, AF.Copy, reads=[pb], writes=[ib])
                    else:
                        cp("dve", io[:, 512:1024], pa, [pb], [ib])
                dmas(queue, f"io{tb % 2}", dram_rows(tb), io, [ib], [dram_buf])

        def load_x(t_, half):
            r0 = t_ * TT + half * 256
            src = x[r0:r0 + 256, :].rearrange("(tb p) d -> p tb d", p=128)
            dmas("sp", f"xin{half}", xin[:, half * 2:half * 2 + 2, :], src, [], [QA if half == 0 else QB])

        for t in range(NT):
            tok0 = t * TT
            tsrc = tabs_d[:, :, tok0:tok0 + TT].rearrange("a p s -> p a s")
            dmas("sp", "tab", tab, tsrc, [], [tabb])
            if t == 0:
                load_x(0, 1)
                load_x(0, 0)
            for tb in (2, 3, 0, 1):
                xb_ = QA if tb < 2 else QB
                for half in range(2):
                    pa, pb = nps()
                    for kk in range(4):
                        k = half * 4 + kk
                        tr(pa[:, kk * 128:(kk + 1) * 128], pb, xin[:, tb, k * 128:(k + 1) * 128], identf, [xb_, identfb])
                    dst = hT[:, half * 4:(half + 1) * 4, tb * 128:(tb + 1) * 128]
                    srcv = v3(pa, 128)
                    wr = [hTb[half * 4 + kk] for kk in range(4)]
                    if half == 0:
                        act(dst, srcv, AF.Copy, reads=[pb], writes=wr)
                    else:
                        cp("dve", dst, srcv, [pb], wr)
                act(sq[:, :, tb * 128:(tb + 1) * 128], hT[:, :, tb * 128:(tb + 1) * 128], AF.Square, reads=hTb, writes=[KS])
            dump(0, t)
            rmsnorm(0)
            for blk in range(4):
                sl, slb = fetch1(t, bl_qk[blk])
                sl3 = v3(sl, 512)
                isq = blk < 2
                for hh in range(2):
                    h = (blk % 2) * 2 + hh
                    p1a, p1b = nps()
                    p2a, p2b = nps()
                    for k in range(8):
                        mm(p1a, p1b, sl3[:, k, (hh * 2) * 128:(hh * 2 + 1) * 128], xnT[:, k, :], k == 0, k == 7, [slb, xnTb[k]])
                    for k in range(8):
                        mm(p2a, p2b, sl3[:, k, (hh * 2 + 1) * 128:(hh * 2 + 2) * 128], xnT[:, k, :], k == 0, k == 7, [slb, xnTb[k]])
                    cos = tab[:, 0 if isq else 2, :]
                    sin = tab[:, 1 if isq else 3, :]
                    dstT = qT if isq else kT
                    dbuf = QA if isq else QB
                    aa, ab, _ = ntmp()
                    ba, bb, _ = ntmp()
                    ca, cb, _ = ntmp()
                    da, db, _ = ntmp()
                    tt("dve", aa, p1a, cos, ALU.mult, [p1b, tabb], [ab])
                    tt("dve", ba, p2a, sin, ALU.mult, [p2b, tabb], [bb])
                    tt("dve", ca, p1a, sin, ALU.mult, [p1b, tabb], [cb])
                    tt("dve", da, p2a, cos, ALU.mult, [p2b, tabb], [db])
                    tt("dve", dstT[:, h * 2, :], aa, ba, ALU.subtract, [ab, bb], [dbuf])
                    tt("dve", dstT[:, h * 2 + 1, :], ca, da, ALU.add, [cb, db], [dbuf])
            def ktok_transposes():
                for tb in range(4):
                    pa, pb = nps()
                    pab = pa.bitcast(BF16)
                    for c8 in range(8):
                        tr(pab[:, c8 * 128:(c8 + 1) * 128], pb, kT[:, c8, tb * 128:(tb + 1) * 128], identb, [QB, identbb])
                    for h in range(H):
                        act(ktok[:, tb, h * 256:(h + 1) * 256], pab[:, h * 256:(h + 1) * 256], AF.Copy,
                            reads=[pb, constb], writes=[KS], scale=kdec[:, h:h + 1])

            for h in range(H):
                sl, slb = fetch1(t, bl_v[h])
                sl3 = v3(sl, 512)
                for tb in range(4):
                    pa, pb = nps()
                    for k in range(8):
                        mm(pa, pb, xnT[:, k, tb * 128:(tb + 1) * 128], sl3[:, k, :], k == 0, k == 7, [slb, xnTb[k]])
                    cp("dve", vtok[:, tb, h * 512:(h + 1) * 512], pa, [pb], [VA])
                if h == 0:
                    ktok_transposes()
            for h in range(H):
                sl, slb = fetch1(t, bl_g[h])
                sl3 = v3(sl, 512)
                for tb in range(4):
                    pa, pb = nps()
                    for k in range(8):
                        mm(pa, pb, xnT[:, k, tb * 128:(tb + 1) * 128], sl3[:, k, :], k == 0, k == 7, [slb, xnTb[k]])
                    act(gtok[:, tb, h * 512:(h + 1) * 512], pa, AF.Silu, reads=[pb], writes=[VB])
            def stageA(c):
                cs = slice(c * 128, (c + 1) * 128)
                psa, psb_ = nps()
                for h in range(H):
                    for dc in range(2):
                        mm(psa[:, h * 128:(h + 1) * 128], psb_, kT[:, h * 2 + dc, cs], qT[:, h * 2 + dc, cs], dc == 0, dc == 1, [QA, QB])
                tt("dve", sT_ap[c % 2], psa, Mt, ALU.mult, [psb_, constb], [sTb[c % 2]])

            def stageB(c):
                cs = slice(c * 128, (c + 1) * 128)
                for h in range(H):
                    if h == 2 and c + 1 < 4:
                        stageA(c + 1)
                    hs = slice(h * 512, (h + 1) * 512)
                    sq_ = ssq4[:, h * 4:(h + 1) * 4]
                    poa, pob = nps()
                    mm(poa, pob, sT_ap[c % 2][:, h * 128:(h + 1) * 128], vtok[:, c, hs], True, False, [sTb[c % 2], VA])
                    for dc in range(2):
                        mm(poa, pob, qT[:, h * 2 + dc, cs], stB[h][:, dc, :], False, dc == 1, [QA, stBb[h][dc]])
                    pts = []
                    for dc in range(2):
                        pta, ptb = nps()
                        mm(pta, ptb, ktok[:, c, h * 256 + dc * 128:h * 256 + (dc + 1) * 128], vtok[:, c, hs], True, True, [KS, VA])
                        pts.append((pta, ptb))
                    act(junk, poa, AF.Square, reads=[pob, constb], writes=[junkb, ssqhb[h]], scale=qdec[:, h:h + 1], accum=sq_[:, 0:1])
                    act(sq_[:, 1:2], sq_[:, 0:1], AF.Sqrt, reads=[ssqhb[h], epsbb], writes=[ssqhb[h]], scale=1.0 / 512, bias=epsb[:, 0:1])
                    for dc in range(2):
                        pta, ptb = pts[dc]
                        stt(stF[h][:, dc, :], stF[h][:, dc, :], float(GAM[h] ** 128), pta, ALU.mult, ALU.add,
                            [stFb[h][dc], ptb], [stFb[h][dc]])
                        act(stB[h][:, dc, :], stF[h][:, dc, :], AF.Copy, reads=[stFb[h][dc]], writes=[stBb[h][dc]])
                    fw.op("dve", (lambda q_: lambda e: e.reciprocal(out=q_[:, 2:3], in_=q_[:, 1:2]))(sq_), reads=[ssqhb[h]], writes=[ssqhb[h]])
                    tt("dve", sq_[:, 3:4], sq_[:, 2:3], qdec[:, h:h + 1], ALU.mult, [ssqhb[h], constb], [ssqhb[h]])
                    stt(gtok[:, c, hs], poa, sq_[:, 3:4], gtok[:, c, hs], ALU.mult, ALU.mult, [pob, ssqhb[h], VB], [VB])

            stageA(0)
            for c in range(4):
                stageB(c)
            for ec2 in range(8):
                pa, pb = nps()
                pab = pa.bitcast(BF16)
                for e2 in range(2):
                    ec = ec2 * 2 + e2
                    for tb in range(4):
                        tr(pab[:, e2 * 512 + tb * 128:e2 * 512 + (tb + 1) * 128], pb, gtok[:, tb, ec * 128:(ec + 1) * 128],
                           identb, [VB, identbb])
                dst = ogT[:, ec2 * 2:ec2 * 2 + 2, :]
                srcv = v3(pab, 512)
                qb = QA if ec2 < 4 else QB
                if ec2 % 2 == 0:
                    act(dst, srcv, AF.Copy, reads=[pb], writes=[qb])
                else:
                    cp("dve", dst, srcv, [pb], [qb])
            for blk in range(4):
                sl, slb = fetch1(t, bl_wo[blk])
                sl3 = v3(sl, 256)
                for dd in range(2):
                    dch = blk * 2 + dd
                    pa, pb = nps()
                    for ek in range(16):
                        mm(pa, pb, sl3[:, ek, dd * 128:(dd + 1) * 128], ogT[:, ek, :], ek == 0, ek == 15,
                           [slb, QA if ek < 8 else QB])
                    resid_add(dch, pa, pb)
            if t + 1 < NT:
                load_x(t + 1, 1)
            dump(1, t)
            rmsnorm(1)
            ffn(lambda j: fetch1(t, bl_ffn[j]), resid_add)
            dump(2, t)
            rmsnorm(2)
            cp("dve", uT[:, :, 0:2], halo, [halob], [VA])
            for half in range(2):
                bc, bh, bb_ = bl_conv[half]
                sc, scb = fetch1(t, bc)
                sh, shb = fetch1(t, bh)
                sc3 = v3(sc, 512)
                sh3 = v3(sh, 512)
                for d4 in range(4):
                    dch = half * 4 + d4
                    pca, pcb = nps()
                    pha, phb = nps()
                    for k in range(8):
                        mm(pca, pcb, sc3[:, k, d4 * 128:(d4 + 1) * 128], xnT[:, k, :], k == 0, k == 7, [scb, xnTb[k]])
                    for k in range(8):
                        mm(pha, phb, sh3[:, k, d4 * 128:(d4 + 1) * 128], xnT[:, k, :], k == 0, k == 7, [shb, xnTb[k]])
                    ta, tb_, _ = ntmp()
                    act(ta, pca, AF.Copy, reads=[pcb], writes=[tb_])
                    tt("dve", uT[:, dch, 2:514], pha, ta, ALU.mult, [phb, tb_], [VA])
                    ya = io_ap[d4 // 2][:, (d4 % 2) * 512:(d4 % 2 + 1) * 512]
                    yb = iob[d4 // 2]
                    ts("dve", ya, uT[:, dch, 0:512], small[:, 40 + dch:41 + dch], ALU.mult, [VA, smallb], [yb])
                    stt(ya, uT[:, dch, 1:513], small[:, 48 + dch:49 + dch], ya, ALU.mult, ALU.add, [VA, smallb, yb], [yb])
                    stt(ya, uT[:, dch, 2:514], small[:, 56 + dch:57 + dch], ya, ALU.mult, ALU.add, [VA, smallb, yb], [yb])
                sb_, sbb = fetch1(t, bb_)
                sb3 = v3(sb_, 512)
                for d4 in range(4):
                    dch = half * 4 + d4
                    pba, pbb = nps()
                    for k in range(8):
                        mm(pba, pbb, sb3[:, k, d4 * 128:(d4 + 1) * 128], xnT[:, k, :], k == 0, k == 7, [sbb, xnTb[k]])
                    ya = io_ap[d4 // 2][:, (d4 % 2) * 512:(d4 % 2 + 1) * 512]
                    yb = iob[d4 // 2]
                    tt("dve", yT[:, dch, :], pba, ya, ALU.mult, [pbb, yb], [QA])
            cp("dve", halo, uT[:, :, 512:514], [VA], [halob])
            for blk in range(2):
                sl, slb = fetch1(t, bl_co[blk])
                sl3 = v3(sl, 512)
                for d4 in range(4):
                    dch = blk * 4 + d4
                    pa, pb = nps()
                    for k in range(8):
                        mm(pa, pb, sl3[:, k, d4 * 128:(d4 + 1) * 128], yT[:, k, :], k == 0, k == 7, [slb, QA])
                    resid_add(dch, pa, pb)
            dump(3, t)
            if t + 1 < NT:
                load_x(t + 1, 0)
            rmsnorm(3, f32out=True)
            store_tokmajor_f32(hT, lambda k: [hTb[k]], lambda tb: h3_scr[tok0 + tb * 128:tok0 + (tb + 1) * 128, :], h3b, "sp")
            for k in range(0, 8, 2):
                act(xnT[:, k:k + 2, :], xnf[:, k:k + 2, :], AF.Copy, reads=[VA], writes=[xnTb[k], xnTb[k + 1]])
            pa, pb = nps()
            for tb in range(4):
                for k in range(8):
                    mm(pa[:, tb * 8:(tb + 1) * 8], pb, xnf[:, k, tb * 128:(tb + 1) * 128], small[:, 64 + k * 8:72 + k * 8],
                       k == 0, k == 7, [VA, smallb])
            cp("dve", lg, pa[:, 0:32], [pb], [lgb])
            for tb in range(4):
                l = lg[:, tb * 8:(tb + 1) * 8]
                m = m8[:, tb * 8:(tb + 1) * 8]
                mask = rt[:, 0:8]
                negm = rt[:, 8:9]
                ex = rt[:, 16:24]
                em = rt[:, 24:32]
                den = rt[:, 32:33]
                gcol = (t * 4 + tb) * 8
                fw.op("dve", (lambda m_, l_: lambda e: e.max(out=m_, in_=l_))(m, l), reads=[lgb], writes=[m8b])
                ts("dve", mask, l, m[:, 1:2], ALU.is_ge, [lgb, m8b], [rtb])
                ts("dve", s1A[:, gcol:gcol + 8], l, m[:, 0:1], ALU.is_equal, [lgb, m8b], [s1b])
                ts("dve", negm, m[:, 0:1], -1.0, ALU.mult, [m8b], [rtb])
                act(ex, l, AF.Exp, reads=[lgb, rtb], writes=[rtb], bias=negm, scale=1.0)
                tt("dve", em, ex, mask, ALU.mult, [rtb], [rtb])
                red(den, em, [rtb], [rtb])
                fw.op("dve", (lambda d_: lambda e: e.reciprocal(out=d_, in_=d_))(den), reads=[rtb], writes=[rtb])
                ts("dve", gA[:, gcol:gcol + 8], em, den, ALU.mult, [rtb], [gAb])
            for tb in range(4):
                pa, pb = nps()
                pab = pa.bitcast(BF16)
                for k in range(8):
                    tr(pab[:, k * 128:(k + 1) * 128], pb, xnT[:, k, tb * 128:(tb + 1) * 128], identb, [xnTb[k], identbb])
                ta, tb_, ti = ntmp()
                tab16 = ta.bitcast(BF16)
                if tb % 2 == 0:
                    act(tab16, pab, AF.Copy, reads=[pb], writes=[tb_])
                else:
                    cp("dve", tab16, pab, [pb], [tb_])
                dmas("sp", f"tm{ti}", xn_scr[tok0 + tb * 128:tok0 + (tb + 1) * 128, :], tab16, [tb_], [xnsb])
        cv_some(len(moe_blocks))
        zf_some(NSLOT * 4)

        W8 = NB * 8
        P2W = [P2] + tmpb + [rstdb, junkb]
        Mf = tmp_ap[0][:, 0:W8]
        csA = tmp_ap[1][:, 0:W8]
        csB = tmp_ap[2][:, 0:W8]
        pos = tmp_ap[3][:, 0:W8]
        dstf = tmp_ap[4][:, 0:W8]
        prod = tmp_ap[5][:, 0:W8]
        m2f = rstd[:, 0:W8]
        Mb16 = junk[:, 0:W8]

        def p2op(fn, extra_r=(), extra_w=()):
            fw.op("dve", fn, reads=[P2, gAb, s1b] + list(extra_r), writes=P2W + list(extra_w))

        p2op(lambda e: e.tensor_single_scalar(out=Mf, in_=gA, scalar=0.0, op=ALU.is_gt))
        p2op(lambda e: e.tensor_copy(out=Mb16, in_=Mf))
        p1a, p1b = nps()
        p2a, p2b = nps()
        mm(p1a[:, 0:W8], p1b, lstrb, Mb16, True, True, [lstrbb, P2])
        mm(p2a[:, 0:W8], p2b, onesb, Mb16, True, True, [onesbb, P2])
        p2op(lambda e: e.tensor_copy(out=csA, in_=p2a[:, 0:W8]), extra_r=[p2b])
        src_, dst_ = csA, csB
        k = 1
        while k < NB:
            s3, d3 = v3(src_, 8), v3(dst_, 8)
            p2op((lambda d3_, s3_, k_: lambda e: e.tensor_tensor(out=d3_[:, k_:, :], in0=s3_[:, k_:, :], in1=s3_[:, :NB - k_, :], op=ALU.add))(d3, s3, k))
            p2op((lambda d3_, s3_, k_: lambda e: e.tensor_copy(out=d3_[:, :k_, :], in_=s3_[:, :k_, :]))(d3, s3, k))
            src_, dst_ = dst_, src_
            k *= 2
        incl = src_
        incl3 = v3(incl, 8)
        pos3 = v3(pos, 8)
        lp3 = v3(p1a[:, 0:W8], 8)
        p2op(lambda e: e.tensor_copy(out=pos3[:, 0:1, :], in_=lp3[:, 0:1, :]), extra_r=[p1b])
        if NB > 1:
            p2op(lambda e: e.tensor_tensor(out=pos3[:, 1:, :], in0=lp3[:, 1:, :], in1=incl3[:, :NB - 1, :], op=ALU.add), extra_r=[p1b])
        n8 = incl3[:, NB - 1, :]
        thr = p2s[:, 0:NT]
        ns8 = p2s[:, 32:40]
        bi8 = p2s[:, 40:48]
        ends8 = p2s[:, 48:56]
        base8 = p2s[:, 56:64]
        sthr = p2s[:, 64:64 + NSLOT]
        es = p2s[:, 112:112 + NSLOT]
        jthr = p2s[:, 160:182]
        pcol = p2s[:, 182:183]
        wb = p2s[:, 184:184 + NSLOT]
        fw.op("pool", lambda e: e.iota(thr, pattern=[[512, NT]], base=0, channel_multiplier=0,
                                       allow_small_or_imprecise_dtypes=True), writes=[P2])
        fw.op("pool", lambda e: e.iota(sthr, pattern=[[512, NSLOT]], base=0, channel_multiplier=0,
                                       allow_small_or_imprecise_dtypes=True), writes=[P2])
        fw.op("pool", lambda e: e.iota(jthr, pattern=[[128, 22]], base=0, channel_multiplier=0,
                                       allow_small_or_imprecise_dtypes=True), writes=[P2])
        fw.op("pool", lambda e: e.iota(pcol, pattern=[[0, 1]], base=NBLK1 * 128, channel_multiplier=1,
                                       allow_small_or_imprecise_dtypes=True), writes=[P2])
        cmp1 = v3(prod[:, 0:8 * NT], NT)
        p2op(lambda e: e.tensor_tensor(out=cmp1, in0=n8.unsqueeze(2).to_broadcast([128, 8, NT]),
                                       in1=thr.unsqueeze(1).to_broadcast([128, 8, NT]), op=ALU.is_gt))
        p2op(lambda e: e.tensor_reduce(out=ns8, in_=cmp1, axis=AX.X, op=ALU.add))
        p2op(lambda e: e.tensor_copy(out=bi8[:, 0:1], in_=ns8[:, 0:1]))
        for e_ in range(1, NE):
            p2op((lambda e_i: lambda e: e.tensor_tensor(out=bi8[:, e_i:e_i + 1], in0=bi8[:, e_i - 1:e_i], in1=ns8[:, e_i:e_i + 1], op=ALU.add))(e_))
        p2op(lambda e: e.tensor_scalar(out=ends8, in0=bi8, scalar1=512.0, scalar2=None, op0=ALU.mult))
        p2op(lambda e: e.scalar_tensor_tensor(out=base8, in0=ns8, scalar=-512.0, in1=ends8, op0=ALU.mult, op1=ALU.add))
        dst3 = v3(dstf, 8)
        p2op(lambda e: e.tensor_tensor(out=dst3, in0=pos3, in1=base8.unsqueeze(1).to_broadcast([128, NB, 8]), op=ALU.add))
        p2op(lambda e: e.tensor_tensor(out=m2f, in0=Mf, in1=s1A, op=ALU.subtract))
        tabi = tab_all.bitcast(I32)
        idx1i = tabi[:, 0:NB]
        idx2i = tabi[:, 64:64 + NB]
        g1 = tab_all[:, 128:128 + NB]
        g2 = tab_all[:, 192:192 + NB]
        widxi = tabi[:, 256:256 + NSLOT * 22]
        assert 256 + NSLOT * 22 <= 2048
        i1f = csA[:, 0:NB]
        i2f = csB[:, 0:NB]
        prod3 = v3(prod, 8)
        p2op(lambda e: e.tensor_tensor(out=prod, in0=s1A, in1=dstf, op=ALU.mult))
        p2op(lambda e: e.tensor_reduce(out=i1f, in_=prod3, axis=AX.X, op=ALU.add))
        p2op(lambda e: e.tensor_copy(out=idx1i, in_=i1f), extra_w=[tabb])
        p2op(lambda e: e.tensor_tensor(out=prod, in0=m2f, in1=dstf, op=ALU.mult))
        p2op(lambda e: e.tensor_reduce(out=i2f, in_=prod3, axis=AX.X, op=ALU.add))
        p2op(lambda e: e.tensor_copy(out=idx2i, in_=i2f), extra_w=[tabb])
        p2op(lambda e: e.tensor_tensor(out=prod, in0=s1A, in1=gA, op=ALU.mult))
        p2op(lambda e: e.tensor_reduce(out=g1, in_=prod3, axis=AX.X, op=ALU.add), extra_w=[tabb])
        p2op(lambda e: e.tensor_tensor(out=prod, in0=m2f, in1=gA, op=ALU.mult))
        p2op(lambda e: e.tensor_reduce(out=g2, in_=prod3, axis=AX.X, op=ALU.add), extra_w=[tabb])
        cmp2 = v3(Mf[:, 0:NSLOT * 8] if NSLOT * 8 <= W8 else tmp_all[:, 0:NSLOT * 8], 8)
        p2op(lambda e: e.tensor_tensor(out=cmp2, in0=ends8.unsqueeze(1).to_broadcast([128, NSLOT, 8]),
                                       in1=sthr.unsqueeze(2).to_broadcast([128, NSLOT, 8]), op=ALU.is_le))
        p2op(lambda e: e.tensor_reduce(out=es, in_=cmp2, axis=AX.X, op=ALU.add))
        p2op(lambda e: e.tensor_scalar(out=es, in0=es, scalar1=float(NE - 1), scalar2=None, op0=ALU.min))
        p2op(lambda e: e.tensor_scalar(out=wb, in0=es, scalar1=float(22 * 128), scalar2=pcol, op0=ALU.mult, op1=ALU.add))
        widxf = v3(tmp_all[:, 1024:1024 + NSLOT * 22], 22)
        p2op(lambda e: e.tensor_tensor(out=widxf, in0=wb.unsqueeze(2).to_broadcast([128, NSLOT, 22]),
                                       in1=jthr.unsqueeze(1).to_broadcast([128, NSLOT, 22]), op=ALU.add))
        p2op(lambda e: e.tensor_copy(out=widxi, in_=tmp_all[:, 1024:1024 + NSLOT * 22]), extra_w=[tabb])
        widx_ref[0] = widxi
        widx_ref[1] = tabb
        barrier[0] = len(seq)

        for bg in range(NB):
            ta, tb_, ti = ntmp()
            stg = ta.bitcast(BF16)
            dmas("sp", f"tm{ti}", stg, xn_scr[bg * 128:(bg + 1) * 128, :], [xnsb], [tb_])
            for idx in (idx1i, idx2i):
                fw.dma("pool", f"sc{ti}", (lambda i_, b_, s_: lambda e: e.indirect_dma_start(
                    out=xg[:, :], out_offset=bass.IndirectOffsetOnAxis(ap=i_[:, b_:b_ + 1], axis=0),
                    in_=s_, in_offset=None))(idx, bg, stg),
                    reads=[tb_, tabb, xgz], writes=[xgs])

        def evac_y(dc, pa, pb):
            if dc % 2 == 0:
                act(hT[:, dc, :], pa, AF.Copy, reads=[pb], writes=[hTb[dc]])
            else:
                cp("dve", hT[:, dc, :], pa, [pb], [hTb[dc]])

        def load_xs(s_):
            dmas("sp", "xs", xs3, xg[s_ * 512:(s_ + 1) * 512, :].rearrange("(tb p) d -> p tb d", p=128), [xgz, xgs], [KS])

        load_xs(0)
        for s in range(NSLOT):
            for kp in range(4):
                pa, pb = nps()
                pab = pa.bitcast(BF16)
                for k2 in range(2):
                    k = kp * 2 + k2
                    for tb in range(4):
                        tr(pab[:, k2 * 512 + tb * 128:k2 * 512 + (tb + 1) * 128], pb, xs3[:, tb, k * 128:(k + 1) * 128],
                           identb, [KS, identbb])
                if kp % 2 == 0:
                    act(xnT[:, kp * 2:kp * 2 + 2, :], v3(pab, 512), AF.Copy, reads=[pb], writes=[xnTb[kp * 2], xnTb[kp * 2 + 1]])
                else:
                    cp("dve", xnT[:, kp * 2:kp * 2 + 2, :], v3(pab, 512), [pb], [xnTb[kp * 2], xnTb[kp * 2 + 1]])
            if s + 1 < NSLOT:
                load_xs(s + 1)
            ffn(lambda j: fetch(("M", s, j)), evac_y)
            store_tokmajor_f32(hT, lambda k: [hTb[k]], lambda tb: yg[s * 512 + tb * 128:s * 512 + (tb + 1) * 128, :], ygb, "sp")

        gfin = hT_all[:, 0:1024]
        dmas("sp", "gf", gfin, gfin_d[:, :], [], [hTb[0], hTb[1]])
        P4 = []
        regs = [(va_f, [VA]), (vb_f, [VB]),
                (arena[:, wsl_off:wsl_off + 4096], [wslb[0], wslb[1]]),
                (arena[:, wsl_off + 4096:wsl_off + 8192], [wslb[2], wslb[3]])]
        NSET = len(regs)
        for si, (reg, rb) in enumerate(regs):
            P4.append([(reg[:, i * 1024:(i + 1) * 1024], Buf(f"p4_{si}_{i}")) for i in range(4)])
        first = [True] * NSET
        exw = {}

        def p4_loads(bg):
            si = bg % NSET
            (h3a, h3bf), (y1a, y1b), (y2a, y2b), (oa, ob) = P4[si]
            ex_w = list(regs[si][1]) if first[si] else []
            first[si] = False
            exw[bg] = ex_w
            dmas("sp", f"a{si}0", h3a, h3_scr[bg * 128:(bg + 1) * 128, :], [h3b], [h3bf] + ex_w)
            for (ya, yb_, idx, key) in ((y1a, y1b, idx1i, f"a{si}1"), (y2a, y2b, idx2i, f"a{si}2")):
                fw.dma("pool", key, (lambda o_, i_, b_: lambda e: e.indirect_dma_start(
                    out=o_, out_offset=None, in_=yg[:, :],
                    in_offset=bass.IndirectOffsetOnAxis(ap=i_[:, b_:b_ + 1], axis=0)))(ya, idx, bg),
                    reads=[ygb, tabb], writes=[yb_] + ex_w)

        for bg in range(min(NSET - 1, NB)):
            p4_loads(bg)
        for bg in range(NB):
            if bg + NSET - 1 < NB:
                p4_loads(bg + NSET - 1)
            si = bg % NSET
            (h3a, h3bf), (y1a, y1b), (y2a, y2b), (oa, ob) = P4[si]
            ex_w = exw[bg]
            sq_ = ssq4[:, (bg % 4) * 4:(bg % 4) * 4 + 4]
            sqb_ = ssqhb[bg % 4]
            stt(h3a, y1a, g1[:, bg:bg + 1], h3a, ALU.mult, ALU.add, [y1b, tabb, h3bf], [h3bf])
            stt(h3a, y2a, g2[:, bg:bg + 1], h3a, ALU.mult, ALU.add, [y2b, tabb, h3bf], [h3bf])
            act(y1a, h3a, AF.Square, reads=[h3bf], writes=[y1b, sqb_], accum=sq_[:, 0:1])
            act(sq_[:, 1:2], sq_[:, 0:1], AF.Sqrt, reads=[sqb_, epsbb], writes=[sqb_], scale=1.0 / D, bias=epsb[:, 0:1])
            fw.op("dve", (lambda q_: lambda e: e.reciprocal(out=q_[:, 2:3], in_=q_[:, 1:2]))(sq_), reads=[sqb_], writes=[sqb_])
            stt(oa, h3a, sq_[:, 2:3], gfin, ALU.mult, ALU.mult, [h3bf, sqb_, hTb[0], hTb[1]], [ob] + ex_w)
            dmas("sp", f"a{si}3", out[bg * 128:(bg + 1) * 128, :], oa, [ob], [outb])

        fw.fence("sp", [P4[i][3][1] for i in range(NSET)] + wslb + iob + [outb])
        if dbg:
            fw.fence("sp", hTb)
        fw.emit_all(st)
        build_nc.stats = {e: len(fw.ops[e]) for e in ENGS}
        build_nc.stats["waits"] = fw.n_waits
    return nc


def host_consts(S):
    consts = np.zeros((128, 520), np.float64)
    j = np.arange(128)[:, None].astype(np.float64)
    i = np.arange(128)[None, :].astype(np.float64)
    for h in range(H):
        g = GAM[h]
        consts[:, h * 128:(h + 1) * 128] = np.where(i >= j, g ** (-(j + 1.0)), 0.0)
        consts[:, 512 + h] = g ** (np.arange(128) + 1.0)
        consts[:, 516 + h] = g ** (127.0 - np.arange(128))
    inv_freq = (np.float32(10000.0) ** (-(np.arange(0, 256, 2, dtype=np.float32)) / np.float32(256))).astype(np.float32)
    pos = np.arange(S, dtype=np.float32)
    ang = (inv_freq[:, None] * pos[None, :]).astype(np.float32).astype(np.float64)
    tabs = np.stack([np.cos(ang), np.sin(ang), np.cos(ang) / 16.0, np.sin(ang) / 16.0]).astype(np.float32)
    return consts.astype(np.float32), tabs


def host_small(inputs):
    small = np.zeros((128, 128), np.float32)
    for n, name in enumerate(["norm_mix0", "norm_ffn0", "norm_mix1", "norm_ffn1", "norm_final"]):
        small[:, n * 8:(n + 1) * 8] = np.asarray(inputs[name], np.float32).reshape(8, 128).T
    cw = np.asarray(inputs["conv_w"], np.float32)
    for jj in range(3):
        small[:, 40 + jj * 8:48 + jj * 8] = cw[jj].reshape(8, 128).T
    wr = np.asarray(inputs["moe_router"], np.float32)
    small[:, 64:128] = wr.reshape(8, 128, 8).transpose(1, 0, 2).reshape(128, 64)
    return small


WNAMES = ["ret_w_in", "ret_w_out", "ffn_w_gate", "ffn_w_up", "ffn_w_down", "conv_w_in", "conv_w_out",
          "moe_w_gate", "moe_w_up", "moe_w_down"]


def make_in_maps(inputs):
    x = np.asarray(inputs["x"], np.float32)
    B, S, _ = x.shape
    consts, tabs = host_consts(S)
    small = host_small(inputs)
    common = {n: np.ascontiguousarray(np.asarray(inputs[n], np.float32)) for n in WNAMES}
    common["small"] = small
    common["consts"] = consts
    common["tabs"] = tabs
    common["gfin"] = np.ascontiguousarray(np.broadcast_to(np.asarray(inputs["norm_final"], np.float32)[None, :], (128, D)))
    return [dict(common, x=np.ascontiguousarray(x[b])) for b in range(B)], B, S


def kernel(**inputs):
    in_maps, B, S = make_in_maps(inputs)
    nc = build_nc(S)
    res = run_bass_kernel_spmd(nc, in_maps, core_ids=list(range(B)))
    return np.stack([np.asarray(r["out"], np.float32) for r in res.results]).astype(np.float32)
```
